# Optimizing a Trainium2 kernel written in Bass

```python
import jax, jax.numpy as jnp
from jax import lax
import numpy as np

D_MODEL = 1024
BATCH = 16
SEQ = 4096
DEPTH = 4

CHUNK = 64
BRANCH_W = 512
N_BRANCH = 3
EPS = 1e-6
SG_BLOCK = 128
SG_GROUPS = 4
SG_GROUP_W = BRANCH_W // SG_GROUPS
MLA_HEADS = 8
MLA_NOPE = 64
MLA_ROPE = 32
MLA_QK = MLA_NOPE + MLA_ROPE
MLA_V = 64
MLA_Q_RANK = 256
MLA_KV_RANK = 128
ROPE_THETA = 10000.0
Q_BLOCK = 128
GLA_HEADS = 4
GLA_DK = 64
GLA_DV = 128
GLA_GATE_RANK = 16
GLA_TAU = 16.0

SPLITS = (BRANCH_W, BRANCH_W, BRANCH_W,
          MLA_Q_RANK, MLA_KV_RANK, MLA_ROPE, MLA_HEADS * MLA_V,
          GLA_HEADS * GLA_DK, GLA_HEADS * GLA_DK, GLA_HEADS * GLA_DV,
          GLA_GATE_RANK, GLA_HEADS * GLA_DV,
          N_BRANCH * D_MODEL)
IN_COLS = sum(SPLITS)

kernel_name = 'hybrid_gmlp_mla_gla_gated_merge'


def _rmsnorm(x, g):
    x32 = x.astype(jnp.float32)
    y = x32 * lax.rsqrt(jnp.mean(x32 * x32, axis=-1, keepdims=True) + EPS)
    return (y * g.astype(jnp.float32)).astype(x.dtype)


def _layernorm(x, g, b):
    x32 = x.astype(jnp.float32)
    mu = jnp.mean(x32, axis=-1, keepdims=True)
    xc = x32 - mu
    y = xc * lax.rsqrt(jnp.mean(xc * xc, axis=-1, keepdims=True) + EPS)
    return (y * g.astype(jnp.float32) + b.astype(jnp.float32)).astype(x.dtype)


def _split_cols(proj):
    idx = np.cumsum(np.array(SPLITS))[:-1].tolist()
    return jnp.split(proj, idx, axis=-1)


def _rope_tables(positions):
    half = MLA_ROPE // 2
    inv = 1.0 / (ROPE_THETA ** (jnp.arange(half, dtype=jnp.float32) * 2.0 / MLA_ROPE))
    ang = positions.astype(jnp.float32)[..., None] * inv
    return jnp.cos(ang)[:, :, None, :], jnp.sin(ang)[:, :, None, :]


def _apply_rope(x, cos, sin):
    half = MLA_ROPE // 2
    x32 = x.astype(jnp.float32)
    x1, x2 = x32[..., :half], x32[..., half:]
    return jnp.concatenate([x1 * cos - x2 * sin, x1 * sin + x2 * cos], axis=-1).astype(x.dtype)


def _spatial_gating(u, v, z, ln_g, ln_b, w_s, b_s):
    bn, s, _ = u.shape
    u = jax.nn.gelu(u)
    v = _layernorm(jax.nn.gelu(v), ln_g, ln_b)
    nb = s // SG_BLOCK
    vb = v.reshape(bn, nb, SG_BLOCK, SG_GROUPS, SG_GROUP_W)
    cid = jnp.arange(SG_BLOCK) // CHUNK
    mask = cid[:, None] >= cid[None, :]
    w = jnp.where(mask[None], w_s, jnp.zeros_like(w_s))
    sv = jnp.einsum('gij,bnjgc->bnigc', w, vb) + b_s.T[None, None, :, :, None]
    return u * sv.reshape(bn, s, BRANCH_W) * jax.nn.silu(z)


def _chunk_causal_attention(q, k, v):
    bn, s, h, dq = q.shape
    dv = v.shape[-1]
    nb = s // Q_BLOCK
    scale = dq ** -0.5
    qb = q.reshape(bn, nb, Q_BLOCK, h, dq).transpose(1, 0, 3, 2, 4)
    kh = k.transpose(0, 2, 1, 3)
    vh = v.transpose(0, 2, 1, 3)
    k_chunk = jnp.arange(s) // CHUNK

    def one_block(args):
        qblk, bi = args
        sc = jnp.einsum('bhqd,bhkd->bhqk', qblk, kh).astype(jnp.float32) * scale
        q_chunk = (bi * Q_BLOCK + jnp.arange(Q_BLOCK)) // CHUNK
        mask = k_chunk[None, :] <= q_chunk[:, None]
        sc = jnp.where(mask, sc, -jnp.inf)
        p = jax.nn.softmax(sc, axis=-1)
        return jnp.einsum('bhqk,bhkd->bhqd', p.astype(vh.dtype), vh)

    out = lax.map(one_block, (qb, jnp.arange(nb)))
    return out.transpose(1, 0, 3, 2, 4).reshape(bn, s, h, dv)


def _mla(c_q, c_kv, k_r, z, cq_g, ckv_g, w_uq, w_ukv, q_g, k_g, cos, sin):
    bn, s, _ = c_q.shape
    q = (_rmsnorm(c_q, cq_g) @ w_uq).reshape(bn, s, MLA_HEADS, MLA_QK)
    kv = (_rmsnorm(c_kv, ckv_g) @ w_ukv).reshape(bn, s, MLA_HEADS, MLA_NOPE + MLA_V)
    k_nope, v = kv[..., :MLA_NOPE], kv[..., MLA_NOPE:]
    k_rope = jnp.broadcast_to(k_r[:, :, None, :], (bn, s, MLA_HEADS, MLA_ROPE))
    k = jnp.concatenate([k_nope, k_rope], axis=-1)
    q = _rmsnorm(q, q_g)
    k = _rmsnorm(k, k_g)
    q = jnp.concatenate([q[..., :MLA_NOPE], _apply_rope(q[..., MLA_NOPE:], cos, sin)], axis=-1)
    k = jnp.concatenate([k[..., :MLA_NOPE], _apply_rope(k[..., MLA_NOPE:], cos, sin)], axis=-1)
    o = _chunk_causal_attention(q, k, v)
    return o.reshape(bn, s, MLA_HEADS * MLA_V) * jax.nn.silu(z)


def _gla(q, k, v, g_lr, z, w_gu, b_gu, o_g):
    bn, s, _ = q.shape
    n = s // CHUNK
    f32 = jnp.float32
    qc = q.astype(f32).reshape(bn, n, CHUNK, GLA_HEADS, GLA_DK) * (GLA_DK ** -0.5)
    kc = k.astype(f32).reshape(bn, n, CHUNK, GLA_HEADS, GLA_DK)
    vc = v.astype(f32).reshape(bn, n, CHUNK, GLA_HEADS, GLA_DV)
    log_a = jax.nn.log_sigmoid((g_lr @ w_gu + b_gu).astype(f32)) / GLA_TAU
    log_a = log_a.reshape(bn, n, CHUNK, GLA_HEADS, GLA_DK)
    b = jnp.cumsum(log_a, axis=2)
    b_last = b[:, :, -1]
    q_t = qc * jnp.exp(b)
    k_t = kc * jnp.exp(-b)
    k_s = kc * jnp.exp(b_last[:, :, None] - b)
    causal = jnp.tril(jnp.ones((CHUNK, CHUNK), dtype=bool))
    att = jnp.einsum('bnihd,bnjhd->bnhij', q_t, k_t)
    att = jnp.where(causal, att, 0.0)
    o_intra = jnp.einsum('bnhij,bnjhe->bnihe', att, vc)

    def step(state, xs):
        q_i, k_i, v_i, dec = xs
        o_i = jnp.einsum('bihd,bhde->bihe', q_i, state)
        new_state = dec[..., None] * state + jnp.einsum('bjhd,bjhe->bhde', k_i, v_i)
        return new_state, o_i

    s0 = jnp.zeros((bn, GLA_HEADS, GLA_DK, GLA_DV), f32)
    xs = (jnp.moveaxis(q_t, 1, 0), jnp.moveaxis(k_s, 1, 0), jnp.moveaxis(vc, 1, 0), jnp.moveaxis(jnp.exp(b_last), 1, 0))
    _, o_inter = lax.scan(step, s0, xs)
    o = (o_intra + jnp.moveaxis(o_inter, 0, 1)).reshape(bn, s, GLA_HEADS, GLA_DV)
    o = _rmsnorm(o, o_g).reshape(bn, s, GLA_HEADS * GLA_DV).astype(z.dtype)
    return o * jax.nn.silu(z)


def setup_inputs(seed: int = 0) -> dict:
    key = jax.random.key(seed)
    ks = jax.random.split(key, 24)
    f32 = jnp.float32
    nrm = lambda k, shp, sc: jax.random.normal(k, shp, f32) * sc
    gain = lambda k, shp: 1.0 + 0.02 * jax.random.normal(k, shp, f32)
    x = jax.random.normal(ks[0], (BATCH, SEQ, D_MODEL), f32)
    offset = jax.random.randint(ks[1], (BATCH, 1), 0, 4096, dtype=jnp.int32)
    positions = offset + jnp.arange(SEQ, dtype=jnp.int32)[None, :]
    return {
        'x': x,
        'positions': positions,
        'norm_g': gain(ks[2], (DEPTH, D_MODEL)),
        'w_in': nrm(ks[3], (DEPTH, D_MODEL, IN_COLS), D_MODEL ** -0.5),
        'b_gate': nrm(ks[4], (DEPTH, N_BRANCH * D_MODEL), 0.1),
        'sg_ln_g': gain(ks[5], (DEPTH, BRANCH_W)),
        'sg_ln_b': nrm(ks[6], (DEPTH, BRANCH_W), 0.02),
        'sg_w': nrm(ks[7], (DEPTH, SG_GROUPS, SG_BLOCK, SG_BLOCK), SG_BLOCK ** -0.5),
        'sg_b': gain(ks[8], (DEPTH, SG_GROUPS, SG_BLOCK)),
        'mla_cq_g': gain(ks[9], (DEPTH, MLA_Q_RANK)),
        'mla_ckv_g': gain(ks[10], (DEPTH, MLA_KV_RANK)),
        'mla_w_uq': nrm(ks[11], (DEPTH, MLA_Q_RANK, MLA_HEADS * MLA_QK), MLA_Q_RANK ** -0.5),
        'mla_w_ukv': nrm(ks[12], (DEPTH, MLA_KV_RANK, MLA_HEADS * (MLA_NOPE + MLA_V)), MLA_KV_RANK ** -0.5),
        'mla_q_g': gain(ks[13], (DEPTH, MLA_QK)),
        'mla_k_g': gain(ks[14], (DEPTH, MLA_QK)),
        'gla_w_gate': nrm(ks[15], (DEPTH, GLA_GATE_RANK, GLA_HEADS * GLA_DK), GLA_GATE_RANK ** -0.5),
        'gla_b_gate': nrm(ks[16], (DEPTH, GLA_HEADS * GLA_DK), 0.1),
        'gla_o_g': gain(ks[17], (DEPTH, GLA_DV)),
        'w_branch': nrm(ks[18], (DEPTH, N_BRANCH, BRANCH_W, D_MODEL), BRANCH_W ** -0.5),
        'w_out': nrm(ks[19], (DEPTH, D_MODEL, D_MODEL), D_MODEL ** -0.5),
    }


def reference(x, positions, norm_g, w_in, b_gate, sg_ln_g, sg_ln_b, sg_w, sg_b, mla_cq_g, mla_ckv_g,
              mla_w_uq, mla_w_ukv, mla_q_g, mla_k_g, gla_w_gate, gla_b_gate, gla_o_g, w_branch, w_out):
    bn, s, d = x.shape
    cos, sin = _rope_tables(positions)
    for l in range(DEPTH):
        h = _rmsnorm(x, norm_g[l])
        (u_a, v_a, z_a, c_q, c_kv, k_r, z_b, q_c, k_c, v_c, g_c, z_c, gate_logits) = _split_cols(h @ w_in[l])
        y_a = _spatial_gating(u_a, v_a, z_a, sg_ln_g[l], sg_ln_b[l], sg_w[l], sg_b[l])
        y_b = _mla(c_q, c_kv, k_r, z_b, mla_cq_g[l], mla_ckv_g[l], mla_w_uq[l], mla_w_ukv[l],
                   mla_q_g[l], mla_k_g[l], cos, sin)
        y_c = _gla(q_c, k_c, v_c, g_c, z_c, gla_w_gate[l], gla_b_gate[l], gla_o_g[l])
        gates = jax.nn.sigmoid(gate_logits + b_gate[l]).reshape(bn, s, N_BRANCH, d)
        merged = (gates[:, :, 0] * (y_a @ w_branch[l, 0])
                  + gates[:, :, 1] * (y_b @ w_branch[l, 1])
                  + gates[:, :, 2] * (y_c @ w_branch[l, 2]))
        x = x + merged @ w_out[l]
    return x
```

```python
import numpy as np
import ml_dtypes
import concourse.bass as bass
import concourse.mybir as mybir
from concourse.bass_utils import run_bass_kernel_spmd

F32 = mybir.dt.float32
BF16 = mybir.dt.bfloat16
I32 = mybir.dt.int32
AF = mybir.ActivationFunctionType
ALU = mybir.AluOpType
AX = mybir.AxisListType

D_MODEL = 1024
IN_COLS = 7088
EPS = 1e-6
N_CORES = 8
PI = 3.141592


class Sem:
    def __init__(self, handle, is_dma):
        self.h = handle
        self.issued = 0
        self.is_dma = is_dma


class V:
    def __init__(self, t, ap):
        self.t = t
        self.ap = ap

    def __getitem__(self, idx):
        return V(self.t, self.ap[idx])

    def re(self, s, **kw):
        return V(self.t, self.ap.rearrange(s, **kw))

    def bc(self, shape):
        return V(self.t, self.ap.to_broadcast(list(shape)))

    def us(self, axis):
        return V(self.t, self.ap.unsqueeze(axis))


class T:
    def __init__(self, ap, name=""):
        self.ap = ap
        self.name = name
        self.w = None
        self.r = {}
        self.dsem = None
        self.is_dram = False

    def __getitem__(self, idx):
        return V(self, self.ap[idx])

    def v(self):
        return V(self, self.ap)

    def re(self, s, **kw):
        return V(self, self.ap.rearrange(s, **kw))


class Eng:
    def __init__(self, h, sem, name, self_sync=True):
        self.h = h
        self.sem = sem
        self.name = name
        self.waited = {}
        self.self_sync = self_sync


class FW:
    def __init__(self, nc):
        self.nc = nc
        self.n_sems = 0
        self.all_sems = []
        mk = lambda h, n, ss=True: Eng(h, self.new_sem(n, False), n, ss)
        self.pe = mk(nc.tensor, "pe", False)
        self.act = mk(nc.scalar, "act")
        self.dve = mk(nc.vector, "dve")
        self.pool = mk(nc.gpsimd, "pool")
        self.sp = mk(nc.sync, "sp")
        self.engs = [self.pe, self.act, self.dve, self.pool, self.sp]
        self.n_inst = 0
        self.n_wait = 0
        self.dsems = {}

    def new_sem(self, name, is_dma):
        self.n_sems += 1
        s = Sem(self.nc.alloc_semaphore(name), is_dma)
        self.all_sems.append(s)
        return s

    def dram(self, ap, name=""):
        t = T(ap, name)
        t.is_dram = True
        return t

    def _wait(self, eng, rd, wr):
        deps = {}

        def add(sv):
            if sv is not None:
                s, v = sv
                if deps.get(s, 0) < v:
                    deps[s] = v
        for t in rd:
            add(t.w)
        for t in wr:
            add(t.w)
            for s, v in t.r.items():
                add((s, v))
        for s, v in deps.items():
            if s.is_dma:
                v = s.issued
            if s is eng.sem and not eng.self_sync:
                continue
            if eng.waited.get(s, 0) >= v:
                continue
            eng.h.wait_ge(s.h, v)
            eng.waited[s] = v
            self.n_wait += 1

    def op(self, eng, fn, rd, wr, inc=True):
        rd = [x.t for x in rd if isinstance(x, V)]
        wr = [x.t for x in wr if isinstance(x, V)]
        self._wait(eng, rd, wr)
        ins = fn()
        self.n_inst += 1
        if inc:
            eng.sem.issued += 1
            ins.then_inc(eng.sem.h, 1)
            v = eng.sem.issued
        else:
            v = eng.sem.issued + 1
        for t in rd:
            t.r[eng.sem] = v
        for t in wr:
            t.w = (eng.sem, v)
            t.r = {}
        return ins

    def dma(self, q, out, in_, sem_t=None, **kw):
        if sem_t is None:
            sem_t = out.t if not out.t.is_dram else (in_.t if not in_.t.is_dram else out.t)
        if sem_t.dsem is None:
            if sem_t.name not in self.dsems:
                self.dsems[sem_t.name] = self.new_sem("d_" + sem_t.name, True)
            sem_t.dsem = self.dsems[sem_t.name]
        s = sem_t.dsem
        self._wait(q, [in_.t], [out.t])
        ins = q.h.dma_start(out=out.ap, in_=in_.ap, **kw)
        s.issued += 16
        ins.then_inc(s.h, 16)
        self.n_inst += 1
        in_.t.r[s] = s.issued
        out.t.w = (s, s.issued)
        out.t.r = {}
        return ins

    def dma_split(self, q, out, in_):
        for a in range(out.ap.shape[1]):
            self.dma(q, out[:, a, :], in_[:, a, :])

    def cast_load(self, dst, src, stages, ctr, eng=None):
        e = eng if eng is not None else self.pool
        np_, na, ncol = dst.ap.shape
        for a in range(na):
            stg = stages[ctr[0] % len(stages)]
            ctr[0] += 1
            self.dma(self.sp, stg[0:np_, 0:ncol], src[:, a, :])
            self.copy(dst[:, a, :], stg[0:np_, 0:ncol], eng=e)

    def wait_all(self, eng, tiles):
        self._wait(eng, list(tiles), [])

    def barrier(self):
        for e in self.engs:
            for s in self.all_sems:
                if s is e.sem or s.issued == 0:
                    continue
                if e.waited.get(s, 0) >= s.issued:
                    continue
                e.h.wait_ge(s.h, s.issued)
                e.waited[s] = s.issued
                self.n_wait += 1

    def mm(self, out, lhsT, rhs, start, stop, inc=None):
        if inc is None:
            inc = stop
        return self.op(self.pe, lambda: self.nc.tensor.matmul(out.ap, lhsT.ap, rhs.ap, start=start, stop=stop),
                       [lhsT, rhs], [out], inc=inc)

    def transpose(self, out, in_, ident, inc=True):
        return self.op(self.pe, lambda: self.nc.tensor.transpose(out.ap, in_.ap, ident.ap),
                       [in_, ident], [out], inc=inc)

    def actf(self, out, in_, func, bias=None, scale=None, accum_out=None):
        kw = {}
        rd = [in_]
        wr = [out]
        if bias is not None:
            kw["bias"] = bias.ap if isinstance(bias, V) else bias
            rd.append(bias)
        if scale is not None:
            kw["scale"] = scale.ap if isinstance(scale, V) else scale
            rd.append(scale)
        if accum_out is not None:
            kw["accum_out"] = accum_out.ap
            wr.append(accum_out)
        return self.op(self.act, lambda: self.nc.scalar.activation(out=out.ap, in_=in_.ap, func=func, **kw), rd, wr)

    def _veng(self, eng):
        return eng if eng is not None else self.dve

    def tt(self, out, in0, in1, op, eng=None):
        e = self._veng(eng)
        return self.op(e, lambda: e.h.tensor_tensor(out=out.ap, in0=in0.ap, in1=in1.ap, op=op), [in0, in1], [out])

    def ts(self, out, in0, s1, op0, s2=None, op1=None, eng=None):
        e = self._veng(eng)
        a1 = s1.ap if isinstance(s1, V) else s1
        a2 = s2.ap if isinstance(s2, V) else s2
        kw = {}
        if op1 is not None:
            kw["op1"] = op1
        return self.op(e, lambda: e.h.tensor_scalar(out=out.ap, in0=in0.ap, scalar1=a1, scalar2=a2, op0=op0, **kw),
                       [in0, s1, s2], [out])

    def stt(self, out, in0, scalar, in1, op0, op1, eng=None):
        e = self._veng(eng)
        a = scalar.ap if isinstance(scalar, V) else scalar
        return self.op(e, lambda: e.h.scalar_tensor_tensor(out=out.ap, in0=in0.ap, scalar=a, in1=in1.ap, op0=op0, op1=op1),
                       [in0, scalar, in1], [out])

    def copy(self, out, in_, eng=None):
        e = self._veng(eng)
        if e is self.act:
            return self.op(e, lambda: self.nc.scalar.copy(out=out.ap, in_=in_.ap), [in_], [out])
        return self.op(e, lambda: e.h.tensor_copy(out=out.ap, in_=in_.ap), [in_], [out])

    def memset(self, out, val, eng=None):
        e = self._veng(eng)
        return self.op(e, lambda: e.h.memset(out.ap, val), [], [out])

    def reduce(self, out, in_, op=ALU.add, axis=AX.X, eng=None):
        e = self._veng(eng)
        return self.op(e, lambda: e.h.tensor_reduce(out=out.ap, in_=in_.ap, axis=axis, op=op), [in_], [out])

    def recip(self, out, in_):
        return self.op(self.dve, lambda: self.nc.vector.reciprocal(out=out.ap, in_=in_.ap), [in_], [out])

    def rsqrt(self, out, in_):
        self.actf(out, in_, AF.Ln)
        self.actf(out, out, AF.Exp, scale=-0.5)


class Arena:
    def __init__(self, ap, nfloats):
        self.ap = ap
        self.n = nfloats
        self.off = 0
        self.marks = []

    def alloc(self, name, shape, dt):
        esz = 4 if dt in (F32, I32) else 2
        free = int(np.prod(shape[1:]))
        nby = free * esz
        nfl = (nby + 31) // 32 * 8
        assert self.off + nfl <= self.n, f"arena overflow at {name}: {self.off + nfl} > {self.n}"
        a = self.ap[:, self.off:self.off + nfl]
        self.off += nfl
        if dt != F32:
            a = a.bitcast(dt)
        a = a[0:shape[0], 0:free]
        if len(shape) == 3:
            a = a.rearrange("p (a b) -> p a b", a=shape[1])
        elif len(shape) == 4:
            a = a.rearrange("p (a b c) -> p a b c", a=shape[1], b=shape[2])
        return T(a, name)

    def mark(self):
        return self.off

    def reset(self, m):
        self.off = m


PP_NORM, PP_CQG, PP_CKVG, PP_BGATE, PP_OG, NPP = 0, 8, 10, 11, 35, 36
BC_LNG, BC_LNB, BC_BGU, BC_QG, BC_KG, BC_SGB, NBC = 0, 512, 1024, 1280, 1376, 1472, 1984

C_UA, C_VA, C_ZA, C_CQ, C_CKV, C_KR, C_ZB, C_QC, C_KC, C_VC, C_GC, C_ZC, C_GATE = (
    0, 512, 1024, 1536, 1792, 1920, 1952, 2464, 2720, 2976, 3488, 3504, 4016)


STOP = None


class _Stop(Exception):
    pass


def chk(name):
    if STOP == name:
        raise _Stop()


def build(nseq, nblk, depth):
    S = nblk * 512
    NT = nseq * nblk * 4
    nc = bass.Bass("TRN2", target_bir_lowering=False)
    fw = FW(nc)

    def din(name, shape, dt=F32):
        return fw.dram(nc.dram_tensor(name, list(shape), dt, kind="ExternalInput").ap(), name)

    X = din("x", [nseq, S, D_MODEL])
    POS = din("posT", [128, NT], I32)
    W_IN = din("w_in", [depth, D_MODEL, IN_COLS])
    W_UQ = din("w_uq", [depth, 256, 768])
    W_UKV = din("w_ukv", [depth, 128, 1024])
    W_GU = din("w_gu", [depth, 16, 256])
    B_GU = din("b_gu", [depth, 1, 256])
    SGWT = din("sg_wT", [depth, 4, 128, 128])
    W_BR = din("w_branch", [depth, 3, 512, 1024])
    W_OUT = din("w_out", [depth, D_MODEL, D_MODEL])
    CPP = din("cpp", [depth, 128, NPP])
    CBC = din("cbc", [depth, 128, NBC])
    CST = din("cst", [128, 128 * 3 + 16])
    Yh = nc.dram_tensor("y", [nseq, S, D_MODEL], F32, kind="ExternalOutput").ap()
    Y = [[fw.dram(Yh, f"y{s}_{t}") for t in range(nblk * 4)] for s in range(nseq)]
    HTh = nc.dram_tensor("ht_scr", [nseq, nblk, 128, 8 * 512], BF16).ap()
    HT = [[fw.dram(HTh, f"ht{s}_{b}") for b in range(nblk)] for s in range(nseq)]
    YBh = nc.dram_tensor("yb_scr", [nseq, nblk, 128, 4 * 512], BF16).ap()
    YB = [[fw.dram(YBh, f"yb{s}_{b}") for b in range(nblk)] for s in range(nseq)]

    NFL = 52400
    arena_h = nc.alloc_sbuf_tensor("arena", [128, NFL], F32)
    AR = Arena(arena_h.ap(), NFL)
    A = AR.alloc

    psW = T(nc.alloc_psum_tensor("psW", [128, 1024], F32).ap(), "psW")
    psT = T(nc.alloc_psum_tensor("psT", [128, 1024], BF16).ap(), "psT")
    psO = T(nc.alloc_psum_tensor("psO", [128, 512], F32).ap(), "psO")
    NPS = 4
    psR = [T(nc.alloc_psum_tensor(f"psR{i}", [128, 512], F32).ap(), f"psR{i}") for i in range(NPS)]
    ps_ctr = [0]

    def ps():
        t = psR[ps_ctr[0] % NPS]
        ps_ctr[0] += 1
        return t

    cst = A("cst", [128, 400], F32)
    ident_b = A("ident_b", [128, 128], BF16)
    Tm_b = A("Tm_b", [128, 128], BF16)
    ones_b = A("ones_b", [128, 128], BF16)
    ones_f = A("ones_f", [128, 128], F32)
    cs = A("cs", [128, NT, 32], F32)
    cpp = A("cpp", [128, NPP], F32)
    cbq = A("cbq", [128, 192], F32)
    wuq = A("wuq", [128, 2, 768], BF16)
    wukv = A("wukv", [128, 1024], BF16)
    sgw = A("sgw", [128, 4, 128], BF16)
    wgu = A("wgu", [16, 256], F32)
    bgu = A("bgu", [1, 256], F32)
    st = A("st", [128, 64], F32)
    Tm_f = cst[:, 128:256]
    Tu_f = cst[:, 256:384]
    invf = cst[:, 384:400]

    fw.dma(fw.sp, cst.v(), CST.v())
    fw.copy(ident_b.v(), cst[:, 0:128])
    fw.copy(Tm_b.v(), Tm_f)
    fw.memset(ones_b.v(), 1.0)
    fw.memset(ones_f.v(), 1.0)
    m0 = AR.mark()
    posi = A("posi", [128, NT], I32)
    posf = A("posf", [128, NT], F32)
    ang = A("ang", [128, NT, 16], F32)
    kf = A("kf", [128, NT, 16], F32)
    ki = A("ki", [128, NT, 16], I32)
    rr = A("rr", [128, NT, 16], F32)
    fw.dma(fw.sp, posi.v(), POS.v())
    fw.copy(posf.v(), posi.v())
    fw.tt(ang.v(), posf.v().us(2).bc([128, NT, 16]), invf.us(1).bc([128, NT, 16]), ALU.mult)
    fw.ts(kf.v(), ang.v(), float(1.0 / (2 * np.pi)), ALU.mult)
    fw.copy(ki.v(), kf.v())
    fw.copy(kf.v(), ki.v())
    C1 = 6.28125
    C2 = float(2 * np.pi - 6.28125)
    fw.stt(rr.v(), kf.v(), -C1, ang.v(), ALU.mult, ALU.add)
    fw.stt(rr.v(), kf.v(), -C2, rr.v(), ALU.mult, ALU.add)
    fw.ts(ang.v(), rr.v(), -PI, ALU.max, PI, ALU.min)
    fw.actf(cs[:, :, 16:32], ang.v(), AF.Sin)
    fw.ts(rr.v(), rr.v(), float(np.pi / 2), ALU.add)
    fw.ts(kf.v(), rr.v(), float(np.pi), ALU.is_gt, float(2 * np.pi), ALU.mult)
    fw.tt(rr.v(), rr.v(), kf.v(), ALU.subtract)
    fw.ts(rr.v(), rr.v(), -PI, ALU.max, PI, ALU.min)
    fw.actf(cs[:, :, 0:16], rr.v(), AF.Sin)
    fw.barrier()
    AR.reset(m0)
    pass_mark = AR.mark()

    sctr = [0]

    def load_layer_consts(l, stages):
        fw.dma(fw.sp, cpp.v(), CPP[l])
        fw.dma(fw.sp, cbq.v(), CBC[l][:, BC_QG:BC_QG + 192])
        fw.cast_load(wuq.v(), W_UQ[l].re("(kc p) c -> p kc c", p=128), stages, sctr)
        fw.cast_load(wukv.v().us(1), W_UKV[l].us(1), stages, sctr)
        fw.cast_load(sgw.v(), SGWT[l].re("g j i -> j g i"), stages, sctr)
        fw.memset(sgw[64:128, :, 0:64], 0.0)
        fw.dma(fw.sp, wgu.v(), W_GU[l])
        fw.dma(fw.sp, bgu.v(), B_GU[l])

    SCALE_A = float(96 ** -0.5)

    def pass1(l, s):
        AR.reset(pass_mark)
        KT = A("KT", [128, 8, S], BF16)
        VC = A("VC", [128, nblk * 4, 8, 66], BF16)
        win1 = A("win1", [128, 8, 928], BF16)
        xr = [A(f"xr{i}", [128, 1024], F32) for i in range(2)]
        hb = A("hb", [128, 1024], BF16)
        junk = A("junk", [128, 1024], BF16)
        hT = A("hT", [128, 8, 512], BF16)
        cqT = A("cqT", [128, 3, 512], BF16)
        zb = A("zb", [128, 4, 512], BF16)
        qT = A("qT", [128, 8, 512], BF16)
        qf = A("qf", [128, 768], F32)
        kr = A("kr", [128, 32], F32)
        qb = A("qb", [128, 8, 96], BF16)
        kb = A("kb", [128, 8, 96], BF16)
        knf = A("knf", [128, 8, 64], F32)
        krn = A("krn", [128, 8, 32], F32)
        rt = [A(f"rt{i}", [128, 8, 16], F32) for i in range(4)]
        pt = [A(f"pt{i}", [128, 512], BF16) for i in range(3)]
        ou = A("ou", [128, 512], F32)
        ytmp = A("ytmp", [128, 512], F32)
        ybT = A("ybT", [128, 4, 512], BF16)

        stages = [A(f"stg{i}", [128, 1024], F32) for i in range(2)]
        if s == 0:
            load_layer_consts(l, stages)
        rden = ou
        sqf = junk
        fw.cast_load(win1.v(), W_IN[l][:, C_CQ:C_CQ + 928].re("(kc p) c -> p kc c", p=128), stages, sctr)
        chk("p1.0a")
        fw.memset(VC[:, :, :, 64:65], 1.0, eng=fw.pool)
        chk("p1.0b")
        src = X if l == 0 else None

        def rope(dst_b, srcf, tix):
            cosb = cs[:, tix, 0:16].us(1).bc([128, 8, 16])
            sinb = cs[:, tix, 16:32].us(1).bc([128, 8, 16])
            x1 = srcf[:, :, 0:16]
            x2 = srcf[:, :, 16:32]
            fw.tt(rt[0].v(), x1, cosb, ALU.mult)
            fw.tt(rt[1].v(), x2, sinb, ALU.mult, eng=fw.pool)
            fw.tt(rt[2].v(), x1, sinb, ALU.mult)
            fw.tt(rt[3].v(), x2, cosb, ALU.mult, eng=fw.pool)
            fw.tt(dst_b[:, :, 0:16], rt[0].v(), rt[1].v(), ALU.subtract)
            fw.tt(dst_b[:, :, 16:32], rt[2].v(), rt[3].v(), ALU.add)

        for b in range(nblk):
            for tl in range(4):
                r0 = b * 512 + tl * 128
                xt = xr[tl % 2]
                xin = X if l == 0 else Y[s][b * 4 + tl]
                fw.dma(fw.sp, xt.v(), xin[s, r0:r0 + 128, :])
                fw.actf(junk.v(), xt.v(), AF.Square, accum_out=st[:, 0:1])
                fw.ts(st[:, 1:2], st[:, 0:1], 1.0 / D_MODEL, ALU.mult, EPS, ALU.add)
                fw.rsqrt(st[:, 1:2], st[:, 1:2])
                fw.ts(hb.v(), xt.v(), st[:, 1:2], ALU.mult)
                chk("p1.1a")
                for kc in range(8):
                    fw.transpose(psT[:, kc * 128:(kc + 1) * 128], hb[:, kc * 128:(kc + 1) * 128], ident_b.v(), inc=(kc == 7))
                fw.tt(hT[:, :, tl * 128:(tl + 1) * 128], psT.re("p (a b) -> p a b", a=8),
                      cpp[:, PP_NORM:PP_NORM + 8].us(2).bc([128, 8, 128]), ALU.mult)
                chk("p1.1b")
            fw.dma(fw.sp, HT[s][b][s, b].re("p (a b) -> p a b", a=8), hT.v())
            chk("p1.1")
            for g in range(3):
                p = ps()
                for kc in range(8):
                    fw.mm(p.v(), win1[:, kc, g * 128:(g + 1) * 128], hT[:, kc, :], kc == 0, kc == 7)
                gcol = cpp[:, PP_CQG + g:PP_CQG + g + 1]
                fw.actf(cqT[:, g, :], p.v(), AF.Copy, scale=gcol)
            for g in range(4):
                p = ps()
                for kc in range(8):
                    fw.mm(p.v(), win1[:, kc, 416 + g * 128:416 + (g + 1) * 128], hT[:, kc, :], kc == 0, kc == 7)
                fw.actf(zb[:, g, :], p.v(), AF.Silu)
            chk("p1.2a")
            for tl in range(4):
                tix = (s * nblk + b) * 4 + tl
                ktile = b * 4 + tl
                tc0 = tl * 128
                p = ps()
                for kc in range(8):
                    fw.mm(p[:, 0:416], hT[:, kc, tc0:tc0 + 128], win1[:, kc, 0:416], kc == 0, kc == 7)
                fw.actf(junk[:, 0:256], p[:, 0:256], AF.Square, accum_out=st[:, 2:3])
                fw.actf(junk[:, 256:384], p[:, 256:384], AF.Square, accum_out=st[:, 3:4])
                fw.copy(kr.v(), p[:, 384:416])
                fw.actf(junk[:, 384:416], kr.v(), AF.Square, accum_out=st[:, 4:5])
                fw.ts(st[:, 5:6], st[:, 2:3], EPS / 256.0, ALU.mult, EPS * EPS, ALU.add)
                fw.ts(st[:, 6:7], st[:, 3:4], 1.0 / 128.0, ALU.mult, EPS, ALU.add)
                fw.rsqrt(st[:, 6:7], st[:, 6:7])
                fw.tt(st[:, 7:8], st[:, 6:7], st[:, 6:7], ALU.mult)
                for kc in range(2):
                    fw.mm(psW[:, 0:512], cqT[:, kc, tc0:tc0 + 128], wuq[:, kc, 0:512], kc == 0, kc == 1)
                for kc in range(2):
                    fw.mm(psW[:, 512:768], cqT[:, kc, tc0:tc0 + 128], wuq[:, kc, 512:768], kc == 0, kc == 1)
                fw.actf(sqf[:, 0:768], psW[:, 0:768], AF.Square)
                fw.reduce(st[:, 8:16], sqf[:, 0:768].re("p (h d) -> p h d", h=8))
                fw.ts(st[:, 8:16], st[:, 8:16], 1.0 / 96.0, ALU.mult, st[:, 5:6], ALU.add)
                fw.rsqrt(st[:, 8:16], st[:, 8:16])
                fw.tt(qf.re("p (h d) -> p h d", h=8), psW[:, 0:768].re("p (h d) -> p h d", h=8),
                      st[:, 8:16].us(2).bc([128, 8, 96]), ALU.mult)
                q3 = qf.re("p (h d) -> p h d", h=8)
                fw.tt(q3, q3, cbq[:, 0:96].us(1).bc([128, 8, 96]), ALU.mult)
                fw.copy(qb[:, :, 0:64], q3[:, :, 0:64], eng=fw.pool)
                rope(qb[:, :, 64:96], q3[:, :, 64:96], tix)
                for h in range(8):
                    fw.transpose(psT[0:96, h * 128:(h + 1) * 128], qb[:, h, :], ident_b.v(), inc=(h == 7))
                fw.copy(qT[0:96, :, tc0:tc0 + 128], psT[0:96, :].re("p (a b) -> p a b", a=8), eng=fw.act)
                fw.mm(psW[:, 0:512], cqT[:, 2, tc0:tc0 + 128], wukv[:, 0:512], True, True)
                fw.mm(psW[:, 512:1024], cqT[:, 2, tc0:tc0 + 128], wukv[:, 512:1024], True, True)
                kv3 = psW.re("p (h d) -> p h d", h=8)
                fw.actf(sqf.v(), psW.v(), AF.Square)
                fw.reduce(st[:, 16:24], sqf.re("p (h d) -> p h d", h=8)[:, :, 0:64])
                fw.stt(st[:, 16:24], st[:, 16:24], st[:, 7:8], st[:, 4:5].bc([128, 8]), ALU.mult, ALU.add)
                fw.ts(st[:, 16:24], st[:, 16:24], 1.0 / 96.0, ALU.mult, EPS, ALU.add)
                fw.rsqrt(st[:, 16:24], st[:, 16:24])
                fw.ts(st[:, 24:32], st[:, 16:24], st[:, 6:7], ALU.mult)
                kg3 = cbq[:, 96:192].us(1).bc([128, 8, 96])
                fw.tt(knf.v(), kv3[:, :, 0:64], st[:, 24:32].us(2).bc([128, 8, 64]), ALU.mult)
                fw.tt(kb[:, :, 0:64], knf.v(), kg3[:, :, 0:64], ALU.mult, eng=fw.pool)
                fw.tt(krn.v(), kr.v().us(1).bc([128, 8, 32]), st[:, 16:24].us(2).bc([128, 8, 32]), ALU.mult)
                fw.tt(krn.v(), krn.v(), kg3[:, :, 64:96], ALU.mult)
                rope(kb[:, :, 64:96], krn.v(), tix)
                fw.ts(VC[:, ktile, :, 0:64], kv3[:, :, 64:128], st[:, 6:7], ALU.mult)
                for h in range(8):
                    fw.transpose(psT[0:96, h * 128:(h + 1) * 128], kb[:, h, :], ident_b.v(), inc=(h == 7))
                fw.copy(KT[0:96, :, ktile * 128:(ktile + 1) * 128], psT[0:96, :].re("p (a b) -> p a b", a=8), eng=fw.act)
            chk("p1.2")
            nk = 4 * b + 4
            pti = 0
            for h in range(8):
                po = psO

                def qk(kt):
                    j = kt - 4 * b
                    c0 = 128 * j if j > 0 else 0
                    p = ps()
                    fw.mm(p[:, c0:512], KT[0:96, h, kt * 128:(kt + 1) * 128], qT[0:96, h, c0:512], True, True)
                    return p, c0
                nxt = qk(0)
                for kt in range(nk):
                    p, c0 = nxt
                    if kt + 1 < nk:
                        nxt = qk(kt + 1)
                    ptile = pt[pti % 3]
                    pti += 1
                    fw.actf(ptile[:, c0:512], p[:, c0:512], AF.Exp, scale=SCALE_A)
                    if kt >= 4 * b:
                        fw.memset(ptile[64:128, c0:c0 + 64], 0.0, eng=fw.pool)
                    fw.mm(po[0:65, c0:512], VC[:, kt, h, 0:65], ptile[:, c0:512], kt == 0, kt == nk - 1)
                fw.recip(rden[64:65, :], po[64:65, :])
                pb = ps()
                fw.mm(pb[0:64, :], ones_f[64:65, 0:64], rden[64:65, :], True, True)
                fw.copy(ou[0:64, :], po[0:64, :], eng=fw.act)
                hp = (h % 2) * 64
                fw.tt(ytmp[hp:hp + 64, :], ou[0:64, :], pb[0:64, :], ALU.mult)
                fw.tt(ybT[hp:hp + 64, h // 2, :], ytmp[hp:hp + 64, :], zb[hp:hp + 64, h // 2, :], ALU.mult, eng=fw.pool)
            fw.dma(fw.sp, YB[s][b][s, b].re("p (a b) -> p a b", a=4), ybT.v())
        fw.barrier()

    def pass2(l, s):
        AR.reset(pass_mark)
        NSLOT = 4
        ring = [A(f"ring{i}", [128, 4096], BF16) for i in range(NSLOT)]
        hT = A("hT2", [128, 8, 512], BF16)
        ybT = A("ybT2", [128, 4, 512], BF16)
        va = A("va", [128, 4, 512], BF16)
        vc = A("vc", [128, 4, 512], BF16)
        kct = A("kct", [128, 4, 256], F32)
        gT = A("gT", [16, 512], F32)
        qk = A("qk", [128, 4, 512], F32)
        ua = A("ua", [128, 4, 512], BF16)
        za = A("za", [128, 4, 512], BF16)
        zc = A("zc", [128, 4, 512], BF16)
        uz = A("uz", [128, 4, 512], BF16)
        gt = A("gt", [128, 24, 512], BF16)
        yaT = A("yaT", [128, 4, 512], BF16)
        ycT = A("ycT", [128, 4, 512], BF16)
        mg = A("mg", [128, 8, 512], BF16)
        xres = [A(f"xres{i}", [128, 1024], F32) for i in range(2)]
        kpad = A("kpad", [128, 4, 128], BF16)
        la = A("la", [128, 256], F32)
        eb = A("eb", [128, 2, 128], F32)
        enb = A("enb", [128, 2, 128], F32)
        qt = A("qt", [128, 2, 128], BF16)
        kt_ = A("kt_", [128, 2, 128], BF16)
        eD = A("eD", [128, 256], F32)
        attm = A("attm", [128, 4, 128], BF16)
        Sf = [A(f"Sf{i}", [128, 128], F32) for i in range(2)]
        Sb = [A(f"Sb{i}", [128, 128], BF16) for i in range(2)]
        gl = A("gl", [128, 512], F32)
        xc = A("xc", [128, 512], F32)
        t1 = A("t1", [128, 512], F32)
        t2 = A("t2", [128, 512], F32)
        t3 = A("t3", [128, 512], F32)
        sqb = A("sqb", [128, 512], BF16)
        junk = A("junk2", [128, 512], BF16)

        stages = [A(f"stg2_{i}", [128, 1024], F32) for i in range(6)]
        cbc = A("cb2", [128, 1280], F32)
        sgb = A("sgb", [128, 512], F32)
        fw.dma(fw.sp, cbc.v(), CBC[l][:, 0:1280])
        fw.dma(fw.sp, sgb.v(), CBC[l][:, BC_SGB:BC_SGB + 512])
        fw.memset(kpad.v(), 0.0, eng=fw.pool)
        for i in range(2):
            fw.memset(Sf[i].v(), 0.0, eng=fw.pool)
            fw.memset(Sb[i].v(), 0.0, eng=fw.pool)

        def win(c0, n):
            return (W_IN[l][:, c0:c0 + n].re("(kc p) c -> p kc c", p=128), 8, n)
        per_block = [win(C_VA, 512), win(C_KC, 512), win(C_KC + 512, 256), win(C_GC, 16), win(C_QC, 512),
                     win(C_UA, 512), win(C_ZA, 512), win(C_ZC, 512)]
        per_block += [win(C_GATE + 512 * i, 512) for i in range(6)]
        per_block += [(W_BR[l, i].re("(kc p) c -> p kc c", p=128), 4, 1024) for i in range(3)]
        per_block += [(W_OUT[l][:, 512 * i:512 * (i + 1)].re("(kc p) c -> p kc c", p=128), 8, 512) for i in range(2)]
        sched = per_block * nblk
        state = {"issued": 0, "next": 0, "done": 0}

        def rel(n=1):
            state["done"] += n
            issue_until(state["done"] + NSLOT)

        def issue_until(n):
            while state["issued"] < min(n, len(sched)):
                i = state["issued"]
                src_v, a, c = sched[i]
                slot = ring[i % NSLOT]
                fw.cast_load(slot[:, 0:a * c].re("p (a c) -> p a c", a=a), src_v, stages, sctr)
                state["issued"] += 1

        def slab():
            i = state["next"]
            assert i < state["done"] + NSLOT
            issue_until(i + 1)
            state["next"] += 1
            _, a, c = sched[i]
            return ring[i % NSLOT][:, 0:a * c].re("p (a c) -> p a c", a=a)

        issue_until(NSLOT)

        def fm_group(w, c0, m=128):
            p = ps()
            for kc in range(8):
                fw.mm(p[0:m, :], w[:, kc, c0:c0 + m], hT[:, kc, :], kc == 0, kc == 7)
            return p

        for b in range(nblk):
            fw.dma(fw.sp, hT.v(), HT[s][b][s, b].re("p (a b) -> p a b", a=8))
            fw.dma(fw.sp, ybT.v(), YB[s][b][s, b].re("p (a b) -> p a b", a=4))
            w = slab()
            for tl in range(4):
                p = ps()
                for kc in range(8):
                    fw.mm(p.v(), hT[:, kc, tl * 128:(tl + 1) * 128], w[:, kc, :], kc == 0, kc == 7)
                fw.actf(gl.v(), p.v(), AF.Gelu, accum_out=st[:, 0:1])
                fw.ts(st[:, 1:2], st[:, 0:1], 1.0 / 512.0, ALU.mult)
                fw.ts(xc.v(), gl.v(), st[:, 1:2], ALU.subtract)
                fw.actf(junk.v(), xc.v(), AF.Square, accum_out=st[:, 2:3])
                fw.ts(st[:, 3:4], st[:, 2:3], 1.0 / 512.0, ALU.mult, EPS, ALU.add)
                fw.rsqrt(st[:, 3:4], st[:, 3:4])
                fw.stt(t1.v(), xc.v(), st[:, 3:4], cbc[:, BC_LNG:BC_LNG + 512], ALU.mult, ALU.mult)
                fw.tt(va[:, tl, :], t1.v(), cbc[:, BC_LNB:BC_LNB + 512], ALU.add, eng=fw.pool)
            rel()
            w = slab()
            for tl in range(4):
                p = ps()
                for kc in range(8):
                    fw.mm(p.v(), hT[:, kc, tl * 128:(tl + 1) * 128], w[:, kc, :], kc == 0, kc == 7)
                fw.copy(kct[:, tl, :], p[:, 0:256], eng=fw.act)
                fw.copy(vc[:, tl, 0:256], p[:, 256:512])
            rel()
            w = slab()
            for tl in range(4):
                p = ps()
                for kc in range(8):
                    fw.mm(p[:, 0:256], hT[:, kc, tl * 128:(tl + 1) * 128], w[:, kc, :], kc == 0, kc == 7)
                fw.copy(vc[:, tl, 256:512], p[:, 0:256], eng=fw.act)
            chk("p2.1")
            rel()
            w = slab()
            p = fm_group(w, 0, 16)
            fw.copy(gT.v(), p[0:16, :])
            rel()
            w = slab()
            for g in range(4):
                p = fm_group(w, g * 128)
                fw.copy(qk[:, g, :], p.v(), eng=fw.act)
            rel()
            w = slab()
            for g in range(4):
                p = fm_group(w, g * 128)
                fw.actf(ua[:, g, :], p.v(), AF.Gelu)
            rel()
            w = slab()
            for g in range(4):
                p = fm_group(w, g * 128)
                fw.actf(za[:, g, :], p.v(), AF.Silu)
            fw.tt(uz.v(), ua.v(), za.v(), ALU.mult, eng=fw.pool)
            rel()
            w = slab()
            for g in range(4):
                p = fm_group(w, g * 128)
                fw.actf(zc[:, g, :], p.v(), AF.Silu)
            for i6 in range(6):
                rel()
                w = slab()
                for g in range(4):
                    gi = i6 * 4 + g
                    p = fm_group(w, g * 128)
                    fw.actf(gt[:, gi, :], p.v(), AF.Sigmoid, bias=cpp[:, PP_BGATE + gi:PP_BGATE + gi + 1])
            chk("p2.2")
            for tl in range(4):
                tc0 = tl * 128
                p = ps()
                for g in range(4):
                    fw.mm(p[:, g * 128:(g + 1) * 128], va[:, tl, g * 128:(g + 1) * 128], sgw[:, g, :], True, True, inc=(g == 3))
                fw.tt(t1.v(), p.v(), sgb.v(), ALU.add)
                fw.tt(yaT[:, :, tc0:tc0 + 128], t1.re("p (g i) -> p g i", g=4), uz[:, :, tc0:tc0 + 128], ALU.mult, eng=fw.pool)
            chk("p2.3")
            for tl in range(4):
                tc0 = tl * 128
                p = ps()
                fw.mm(p[:, 0:256], gT[0:16, tc0:tc0 + 128], wgu[0:16, :], True, False, inc=False)
                fw.mm(p[:, 0:256], ones_f[0:1, 0:128], bgu[0:1, :], False, True)
                fw.actf(la.v(), p[:, 0:256], AF.Exp, scale=-1.0)
                fw.actf(la.v(), la.v(), AF.Ln, bias=1.0)
                fw.ts(la.v(), la.v(), -1.0 / 16.0, ALU.mult)
                chk("g1")
                pbT = ps()
                for fc in range(2):
                    fw.mm(pbT[:, fc * 128:(fc + 1) * 128], la[:, fc * 128:(fc + 1) * 128], Tm_f, True, True, inc=(fc == 1))
                pD = ps()
                fw.mm(pD[:, 0:256], Tu_f, la.v(), True, True)
                fw.actf(eb.re("p a b -> p (a b)"), pbT[:, 0:256], AF.Exp)
                fw.actf(enb.re("p a b -> p (a b)"), pbT[:, 0:256], AF.Exp, scale=-1.0)
                fw.actf(eD.v(), pD[:, 0:256], AF.Exp)
                chk("g2")
                fw.stt(qt.v(), qk[:, 0:2, tc0:tc0 + 128], 0.125, eb.v(), ALU.mult, ALU.mult)
                fw.tt(kt_.v(), qk[:, 2:4, tc0:tc0 + 128], enb.v(), ALU.mult, eng=fw.pool)
                for h in range(4):
                    o = (h % 2) * 64
                    fw.tt(kpad[:, h, o:o + 64], kct[:, tl, h * 64:(h + 1) * 64], eD[:, h * 64:(h + 1) * 64], ALU.mult,
                          eng=(fw.pool if h % 2 else fw.dve))
                chk("g3")
                po = psO
                for h in range(4):
                    fc = h // 2
                    o = (h % 2) * 64
                    pa = ps()
                    fw.mm(pa[:, 0:128], kt_[o:o + 64, fc, :], qt[o:o + 64, fc, :], True, True)
                    fw.tt(attm[:, h, :], pa[:, 0:128], Tm_b.v(), ALU.mult)
                chk("g4")
                for c in range(2):
                    r0 = c * 64
                    for h in range(4):
                        fc = h // 2
                        o = (h % 2) * 64
                        dst = po[:, h * 128 + r0:h * 128 + r0 + 64]
                        fw.mm(dst, vc[:, tl, h * 128:(h + 1) * 128], attm[:, h, r0:r0 + 64], True, False, inc=False)
                        fw.mm(dst, Sb[fc][o:o + 64, :], qt[o:o + 64, fc, r0:r0 + 64], False, True, inc=True)
                    chk("g4b")
                    for fc in range(2):
                        pk = ps()
                        for hh in range(2):
                            h = fc * 2 + hh
                            fw.mm(pk[:, 0:128], kpad[r0:r0 + 64, h, :], vc[r0:r0 + 64, tl, h * 128:(h + 1) * 128], hh == 0, hh == 1)
                        dec = eb[:, fc, r0 + 63:r0 + 64]
                        fw.stt(Sf[fc].v(), Sf[fc].v(), dec, pk[:, 0:128], ALU.mult, ALU.add)
                        fw.copy(Sb[fc].v(), Sf[fc].v(), eng=fw.act)
                chk("g5")
                fw.actf(sqb.v(), po.v(), AF.Square)
                pss = ps()
                fw.mm(pss.v(), ones_b.v(), sqb.v(), True, True)
                fw.ts(t2.v(), pss.v(), 1.0 / 128.0, ALU.mult, EPS, ALU.add)
                fw.rsqrt(t2.v(), t2.v())
                fw.tt(t3.v(), po.v(), t2.v(), ALU.mult)
                fw.stt(ycT[:, :, tc0:tc0 + 128], t3.re("p (h i) -> p h i", h=4), cpp[:, PP_OG:PP_OG + 1],
                       zc[:, :, tc0:tc0 + 128], ALU.mult, ALU.mult)
            chk("gla")
            rel()
            wb = [slab() for _ in range(3)]
            ys = [yaT, ybT, ycT]
            for og in range(8):
                pp = []
                for i in range(3):
                    p = ps()
                    for kc in range(4):
                        fw.mm(p.v(), wb[i][:, kc, og * 128:(og + 1) * 128], ys[i][:, kc, :], kc == 0, kc == 3)
                    pp.append(p)
                fw.tt(t1.v(), pp[0].v(), gt[:, og, :], ALU.mult)
                fw.tt(t2.v(), pp[1].v(), gt[:, 8 + og, :], ALU.mult)
                fw.tt(t3.v(), pp[2].v(), gt[:, 16 + og, :], ALU.mult)
                fw.tt(t1.v(), t1.v(), t2.v(), ALU.add, eng=fw.pool)
                fw.tt(mg[:, og, :], t1.v(), t3.v(), ALU.add, eng=fw.pool)
            chk("p2.4")
            rel(3)
            wo = [slab() for _ in range(2)]
            for tl in range(4):
                r0 = b * 512 + tl * 128
                xt = xres[tl % 2]
                xin = X if l == 0 else Y[s][b * 4 + tl]
                fw.dma(fw.sp, xt.v(), xin[s, r0:r0 + 128, :])
                for hf in range(2):
                    p = ps()
                    for kc in range(8):
                        fw.mm(p.v(), mg[:, kc, tl * 128:(tl + 1) * 128], wo[hf][:, kc, :], kc == 0, kc == 7)
                    fw.tt(xt[:, hf * 512:(hf + 1) * 512], xt[:, hf * 512:(hf + 1) * 512], p.v(), ALU.add)
                fw.dma(fw.sp, Y[s][b * 4 + tl][s, r0:r0 + 128, :], xt.v())
            rel(2)
        fw.barrier()

    try:
        chk("setup")
        for l in range(depth):
            for s in range(nseq):
                pass1(l, s)
                chk("pass1")
                pass2(l, s)
    except _Stop:
        pass
    fw.barrier()
    return nc, fw


def _consts():
    j = np.arange(128)[:, None]
    i = np.arange(128)[None, :]
    same = (j // 64) == (i // 64)
    Tm = (same & (j <= i)).astype(np.float32)
    Tu = (same & (j > i)).astype(np.float32)
    half = 16
    inv = (np.float32(1.0) / np.power(np.float32(10000.0),
                                      np.arange(half, dtype=np.float32) * np.float32(2.0) / np.float32(32))).astype(np.float32)
    cst = np.concatenate([np.eye(128, dtype=np.float32), Tm, Tu, np.broadcast_to(inv[None, :], (128, 16))], axis=1)
    return np.ascontiguousarray(cst)


def _pack(inputs, depth):
    f = lambda k: np.asarray(inputs[k], dtype=np.float32)
    cpp = np.zeros((depth, 128, NPP), np.float32)
    cbc = np.zeros((depth, 128, NBC), np.float32)
    for l in range(depth):
        cpp[l, :, PP_NORM:PP_NORM + 8] = f("norm_g")[l].reshape(8, 128).T
        cpp[l, :, PP_CQG:PP_CQG + 2] = f("mla_cq_g")[l].reshape(2, 128).T
        cpp[l, :, PP_CKVG] = f("mla_ckv_g")[l]
        cpp[l, :, PP_BGATE:PP_BGATE + 24] = f("b_gate")[l].reshape(24, 128).T
        cpp[l, :, PP_OG] = f("gla_o_g")[l]
        cbc[l, :, BC_LNG:BC_LNG + 512] = f("sg_ln_g")[l][None, :]
        cbc[l, :, BC_LNB:BC_LNB + 512] = f("sg_ln_b")[l][None, :]
        cbc[l, :, BC_BGU:BC_BGU + 256] = f("gla_b_gate")[l][None, :]
        cbc[l, :, BC_QG:BC_QG + 96] = f("mla_q_g")[l][None, :]
        cbc[l, :, BC_KG:BC_KG + 96] = f("mla_k_g")[l][None, :]
        cbc[l, :, BC_SGB:BC_SGB + 512] = f("sg_b")[l].reshape(512)[None, :]
    return cpp, cbc


_CACHE = {}


def run(inputs, n_cores, nseq, nblk, depth):
    key = (nseq, nblk, depth)
    if key not in _CACHE:
        _CACHE[key] = build(nseq, nblk, depth)
    nc, fw = _CACHE[key]
    S = nblk * 512
    x = np.asarray(inputs["x"], np.float32)
    pos = np.asarray(inputs["positions"], np.int32)
    cpp, cbc = _pack(inputs, depth)
    cst = _consts()
    f = lambda k: np.ascontiguousarray(np.asarray(inputs[k], np.float32)[:depth])
    shared = {
        "w_in": f("w_in"), "w_uq": f("mla_w_uq"), "w_ukv": f("mla_w_ukv"), "w_gu": f("gla_w_gate"),
        "b_gu": f("gla_b_gate").reshape(depth, 1, 256),
        "sg_wT": np.ascontiguousarray(f("sg_w").transpose(0, 1, 3, 2)),
        "w_branch": f("w_branch"), "w_out": f("w_out"), "cpp": cpp, "cbc": cbc, "cst": cst,
    }
    in_maps = []
    for c in range(n_cores):
        xs = np.ascontiguousarray(x[c * nseq:(c + 1) * nseq, :S])
        ps_ = pos[c * nseq:(c + 1) * nseq, :S]
        posT = np.ascontiguousarray(ps_.reshape(nseq * nblk * 4, 128).T)
        m = dict(shared)
        m["x"] = xs
        m["posT"] = posT
        in_maps.append(m)
    res = run_bass_kernel_spmd(nc, in_maps, core_ids=list(range(n_cores)))
    return np.concatenate([r["y"] for r in res.results], axis=0)


def kernel(**inputs):
    return run(inputs, N_CORES, 2, 8, 4)
```

```python
import numpy as np
import ml_dtypes
import concourse.bass as bass
import concourse.mybir as mybir
from concourse.bass_utils import run_bass_kernel_spmd

F32 = mybir.dt.float32
BF16 = mybir.dt.bfloat16
I32 = mybir.dt.int32
AF = mybir.ActivationFunctionType
ALU = mybir.AluOpType
AX = mybir.AxisListType

D_MODEL = 1024
IN_COLS = 7088
EPS = 1e-6
N_CORES = 8
PI = 3.141592


class Sem:
    def __init__(self, handle, is_dma):
        self.h = handle
        self.issued = 0
        self.is_dma = is_dma


class V:
    def __init__(self, t, ap):
        self.t = t
        self.ap = ap

    def __getitem__(self, idx):
        return V(self.t, self.ap[idx])

    def re(self, s, **kw):
        return V(self.t, self.ap.rearrange(s, **kw))

    def bc(self, shape):
        return V(self.t, self.ap.to_broadcast(list(shape)))

    def us(self, axis):
        return V(self.t, self.ap.unsqueeze(axis))


class T:
    def __init__(self, ap, name=""):
        self.ap = ap
        self.name = name
        self.w = None
        self.r = {}
        self.dsem = None
        self.is_dram = False

    def __getitem__(self, idx):
        return V(self, self.ap[idx])

    def v(self):
        return V(self, self.ap)

    def re(self, s, **kw):
        return V(self, self.ap.rearrange(s, **kw))


class Eng:
    def __init__(self, h, sem, name, self_sync=True):
        self.h = h
        self.sem = sem
        self.name = name
        self.waited = {}
        self.self_sync = self_sync


class FW:
    def __init__(self, nc):
        self.nc = nc
        self.n_sems = 0
        self.all_sems = []
        mk = lambda h, n, ss=True: Eng(h, self.new_sem(n, False), n, ss)
        self.pe = mk(nc.tensor, "pe", False)
        self.act = mk(nc.scalar, "act")
        self.dve = mk(nc.vector, "dve")
        self.pool = mk(nc.gpsimd, "pool")
        self.sp = mk(nc.sync, "sp")
        self.engs = [self.pe, self.act, self.dve, self.pool, self.sp]
        self.n_inst = 0
        self.n_wait = 0
        self.dsems = {}

    def new_sem(self, name, is_dma):
        self.n_sems += 1
        s = Sem(self.nc.alloc_semaphore(name), is_dma)
        self.all_sems.append(s)
        return s

    def dram(self, ap, name=""):
        t = T(ap, name)
        t.is_dram = True
        return t

    def _wait(self, eng, rd, wr):
        deps = {}

        def add(sv):
            if sv is not None:
                s, v = sv
                if deps.get(s, 0) < v:
                    deps[s] = v
        for t in rd:
            add(t.w)
        for t in wr:
            add(t.w)
            for s, v in t.r.items():
                add((s, v))
        for s, v in deps.items():
            if s.is_dma:
                v = s.issued
            if s is eng.sem and not eng.self_sync:
                continue
            if eng.waited.get(s, 0) >= v:
                continue
            eng.h.wait_ge(s.h, v)
            eng.waited[s] = v
            self.n_wait += 1

    def op(self, eng, fn, rd, wr, inc=True):
        rd = [x.t for x in rd if isinstance(x, V)]
        wr = [x.t for x in wr if isinstance(x, V)]
        self._wait(eng, rd, wr)
        ins = fn()
        self.n_inst += 1
        if inc:
            eng.sem.issued += 1
            ins.then_inc(eng.sem.h, 1)
            v = eng.sem.issued
        else:
            v = eng.sem.issued + 1
        for t in rd:
            t.r[eng.sem] = v
        for t in wr:
            t.w = (eng.sem, v)
            t.r = {}
        return ins

    def dma(self, q, out, in_, sem_t=None, **kw):
        if sem_t is None:
            sem_t = out.t if not out.t.is_dram else (in_.t if not in_.t.is_dram else out.t)
        if sem_t.dsem is None:
            if sem_t.name not in self.dsems:
                self.dsems[sem_t.name] = self.new_sem("d_" + sem_t.name, True)
            sem_t.dsem = self.dsems[sem_t.name]
        s = sem_t.dsem
        self._wait(q, [in_.t], [out.t])
        ins = q.h.dma_start(out=out.ap, in_=in_.ap, **kw)
        s.issued += 16
        ins.then_inc(s.h, 16)
        self.n_inst += 1
        in_.t.r[s] = s.issued
        out.t.w = (s, s.issued)
        out.t.r = {}
        return ins

    def dma_split(self, q, out, in_):
        for a in range(out.ap.shape[1]):
            self.dma(q, out[:, a, :], in_[:, a, :])

    def cast_load(self, dst, src, stages, ctr, eng=None):
        e = eng if eng is not None else self.pool
        np_, na, ncol = dst.ap.shape
        for a in range(na):
            stg = stages[ctr[0] % len(stages)]
            ctr[0] += 1
            self.dma(self.sp, stg[0:np_, 0:ncol], src[:, a, :])
            self.copy(dst[:, a, :], stg[0:np_, 0:ncol], eng=e)

    def wait_all(self, eng, tiles):
        self._wait(eng, list(tiles), [])

    def barrier(self):
        for e in self.engs:
            for s in self.all_sems:
                if s is e.sem or s.issued == 0:
                    continue
                if e.waited.get(s, 0) >= s.issued:
                    continue
                e.h.wait_ge(s.h, s.issued)
                e.waited[s] = s.issued
                self.n_wait += 1

    def mm(self, out, lhsT, rhs, start, stop, inc=None):
        if inc is None:
            inc = stop
        return self.op(self.pe, lambda: self.nc.tensor.matmul(out.ap, lhsT.ap, rhs.ap, start=start, stop=stop),
                       [lhsT, rhs], [out], inc=inc)

    def transpose(self, out, in_, ident, inc=True):
        return self.op(self.pe, lambda: self.nc.tensor.transpose(out.ap, in_.ap, ident.ap),
                       [in_, ident], [out], inc=inc)

    def actf(self, out, in_, func, bias=None, scale=None, accum_out=None):
        kw = {}
        rd = [in_]
        wr = [out]
        if bias is not None:
            kw["bias"] = bias.ap if isinstance(bias, V) else bias
            rd.append(bias)
        if scale is not None:
            kw["scale"] = scale.ap if isinstance(scale, V) else scale
            rd.append(scale)
        if accum_out is not None:
            kw["accum_out"] = accum_out.ap
            wr.append(accum_out)
        return self.op(self.act, lambda: self.nc.scalar.activation(out=out.ap, in_=in_.ap, func=func, **kw), rd, wr)

    def _veng(self, eng):
        return eng if eng is not None else self.dve

    def tt(self, out, in0, in1, op, eng=None):
        e = self._veng(eng)
        return self.op(e, lambda: e.h.tensor_tensor(out=out.ap, in0=in0.ap, in1=in1.ap, op=op), [in0, in1], [out])

    def ts(self, out, in0, s1, op0, s2=None, op1=None, eng=None):
        e = self._veng(eng)
        a1 = s1.ap if isinstance(s1, V) else s1
        a2 = s2.ap if isinstance(s2, V) else s2
        kw = {}
        if op1 is not None:
            kw["op1"] = op1
        return self.op(e, lambda: e.h.tensor_scalar(out=out.ap, in0=in0.ap, scalar1=a1, scalar2=a2, op0=op0, **kw),
                       [in0, s1, s2], [out])

    def stt(self, out, in0, scalar, in1, op0, op1, eng=None):
        e = self._veng(eng)
        a = scalar.ap if isinstance(scalar, V) else scalar
        return self.op(e, lambda: e.h.scalar_tensor_tensor(out=out.ap, in0=in0.ap, scalar=a, in1=in1.ap, op0=op0, op1=op1),
                       [in0, scalar, in1], [out])

    def copy(self, out, in_, eng=None):
        e = self._veng(eng)
        if e is self.act:
            return self.op(e, lambda: self.nc.scalar.copy(out=out.ap, in_=in_.ap), [in_], [out])
        return self.op(e, lambda: e.h.tensor_copy(out=out.ap, in_=in_.ap), [in_], [out])

    def memset(self, out, val, eng=None):
        e = self._veng(eng)
        return self.op(e, lambda: e.h.memset(out.ap, val), [], [out])

    def reduce(self, out, in_, op=ALU.add, axis=AX.X, eng=None):
        e = self._veng(eng)
        return self.op(e, lambda: e.h.tensor_reduce(out=out.ap, in_=in_.ap, axis=axis, op=op), [in_], [out])

    def recip(self, out, in_):
        return self.op(self.dve, lambda: self.nc.vector.reciprocal(out=out.ap, in_=in_.ap), [in_], [out])

    def rsqrt(self, out, in_):
        self.actf(out, in_, AF.Ln)
        self.actf(out, out, AF.Exp, scale=-0.5)


class Arena:
    def __init__(self, ap, nfloats):
        self.ap = ap
        self.n = nfloats
        self.off = 0
        self.marks = []

    def alloc(self, name, shape, dt):
        esz = 4 if dt in (F32, I32) else 2
        free = int(np.prod(shape[1:]))
        nby = free * esz
        nfl = (nby + 31) // 32 * 8
        assert self.off + nfl <= self.n, f"arena overflow at {name}: {self.off + nfl} > {self.n}"
        a = self.ap[:, self.off:self.off + nfl]
        self.off += nfl
        if dt != F32:
            a = a.bitcast(dt)
        a = a[0:shape[0], 0:free]
        if len(shape) == 3:
            a = a.rearrange("p (a b) -> p a b", a=shape[1])
        elif len(shape) == 4:
            a = a.rearrange("p (a b c) -> p a b c", a=shape[1], b=shape[2])
        return T(a, name)

    def mark(self):
        return self.off

    def reset(self, m):
        self.off = m


PP_NORM, PP_CQG, PP_CKVG, PP_BGATE, PP_OG, NPP = 0, 8, 10, 11, 35, 36
BC_LNG, BC_LNB, BC_BGU, BC_QG, BC_KG, BC_SGB, NBC = 0, 512, 1024, 1280, 1376, 1472, 1984

C_UA, C_VA, C_ZA, C_CQ, C_CKV, C_KR, C_ZB, C_QC, C_KC, C_VC, C_GC, C_ZC, C_GATE = (
    0, 512, 1024, 1536, 1792, 1920, 1952, 2464, 2720, 2976, 3488, 3504, 4016)


STOP = None


class _Stop(Exception):
    pass


def chk(name):
    if STOP == name:
        raise _Stop()


def build(nseq, nblk, depth):
    S = nblk * 512
    NT = nseq * nblk * 4
    nc = bass.Bass("TRN2", target_bir_lowering=False)
    fw = FW(nc)

    def din(name, shape, dt=F32):
        return fw.dram(nc.dram_tensor(name, list(shape), dt, kind="ExternalInput").ap(), name)

    X = din("x", [nseq, S, D_MODEL])
    POS = din("posT", [128, NT], I32)
    W_IN = din("w_in", [depth, D_MODEL, IN_COLS])
    W_UQ = din("w_uq", [depth, 256, 768])
    W_UKV = din("w_ukv", [depth, 128, 1024])
    W_GU = din("w_gu", [depth, 16, 256])
    B_GU = din("b_gu", [depth, 1, 256])
    SGWT = din("sg_wT", [depth, 4, 128, 128])
    W_BR = din("w_branch", [depth, 3, 512, 1024])
    W_OUT = din("w_out", [depth, D_MODEL, D_MODEL])
    CPP = din("cpp", [depth, 128, NPP])
    CBC = din("cbc", [depth, 128, NBC])
    CST = din("cst", [128, 128 * 3 + 16])
    Yh = nc.dram_tensor("y", [nseq, S, D_MODEL], F32, kind="ExternalOutput").ap()
    Y = [[fw.dram(Yh, f"y{s}_{t}") for t in range(nblk * 4)] for s in range(nseq)]
    HTh = nc.dram_tensor("ht_scr", [nseq, nblk, 128, 8 * 512], BF16).ap()
    HT = [[fw.dram(HTh, f"ht{s}_{b}") for b in range(nblk)] for s in range(nseq)]
    YBh = nc.dram_tensor("yb_scr", [nseq, nblk, 128, 4 * 512], BF16).ap()
    YB = [[fw.dram(YBh, f"yb{s}_{b}") for b in range(nblk)] for s in range(nseq)]

    NSL = 19
    WSh = nc.dram_tensor("ws_scr", [NSL, 128, 4096], BF16).ap()
    WS = [fw.dram(WSh, f"ws{i}") for i in range(NSL)]
    W1h = nc.dram_tensor("w1_scr", [128, 7424], BF16).ap()
    W1 = fw.dram(W1h, "w1")
    NFL = 52400
    arena_h = nc.alloc_sbuf_tensor("arena", [128, NFL], F32)
    AR = Arena(arena_h.ap(), NFL)
    A = AR.alloc

    psW = T(nc.alloc_psum_tensor("psW", [128, 1024], F32).ap(), "psW")
    psT = T(nc.alloc_psum_tensor("psT", [128, 1024], BF16).ap(), "psT")
    psO = T(nc.alloc_psum_tensor("psO", [128, 512], F32).ap(), "psO")
    NPS = 4
    psR = [T(nc.alloc_psum_tensor(f"psR{i}", [128, 512], F32).ap(), f"psR{i}") for i in range(NPS)]
    def ring_alloc(tiles):
        ctr = [0]

        def f():
            t = tiles[ctr[0] % len(tiles)]
            ctr[0] += 1
            return t
        return f
    ps = ring_alloc(psR)
    psWa = T(psW.ap[:, 0:512], "psWa")
    psWb = T(psW.ap[:, 512:1024], "psWb")

    cst = A("cst", [128, 400], F32)
    ident_b = A("ident_b", [128, 128], BF16)
    Tm_b = A("Tm_b", [128, 128], BF16)
    ones_b = A("ones_b", [128, 128], BF16)
    ones_f = A("ones_f", [128, 128], F32)
    cs = A("cs", [128, NT, 32], F32)
    cpp = A("cpp", [128, NPP], F32)
    cbq = A("cbq", [128, 192], F32)
    wuq = A("wuq", [128, 2, 768], BF16)
    wukv = A("wukv", [128, 1024], BF16)
    sgw = A("sgw", [128, 4, 128], BF16)
    wgu = A("wgu", [16, 256], F32)
    bgu = A("bgu", [1, 256], F32)
    st = A("st", [128, 64], F32)
    Tm_f = cst[:, 128:256]
    Tu_f = cst[:, 256:384]
    invf = cst[:, 384:400]

    fw.dma(fw.sp, cst.v(), CST.v())
    fw.copy(ident_b.v(), cst[:, 0:128])
    fw.copy(Tm_b.v(), Tm_f)
    fw.memset(ones_b.v(), 1.0)
    fw.memset(ones_f.v(), 1.0)
    m0 = AR.mark()
    posi = A("posi", [128, NT], I32)
    posf = A("posf", [128, NT], F32)
    ang = A("ang", [128, NT, 16], F32)
    kf = A("kf", [128, NT, 16], F32)
    ki = A("ki", [128, NT, 16], I32)
    rr = A("rr", [128, NT, 16], F32)
    fw.dma(fw.sp, posi.v(), POS.v())
    fw.copy(posf.v(), posi.v())
    fw.tt(ang.v(), posf.v().us(2).bc([128, NT, 16]), invf.us(1).bc([128, NT, 16]), ALU.mult)
    fw.ts(kf.v(), ang.v(), float(1.0 / (2 * np.pi)), ALU.mult)
    fw.copy(ki.v(), kf.v())
    fw.copy(kf.v(), ki.v())
    C1 = 6.28125
    C2 = float(2 * np.pi - 6.28125)
    fw.stt(rr.v(), kf.v(), -C1, ang.v(), ALU.mult, ALU.add)
    fw.stt(rr.v(), kf.v(), -C2, rr.v(), ALU.mult, ALU.add)
    fw.ts(ang.v(), rr.v(), -PI, ALU.max, PI, ALU.min)
    fw.actf(cs[:, :, 16:32], ang.v(), AF.Sin)
    fw.ts(rr.v(), rr.v(), float(np.pi / 2), ALU.add)
    fw.ts(kf.v(), rr.v(), float(np.pi), ALU.is_gt, float(2 * np.pi), ALU.mult)
    fw.tt(rr.v(), rr.v(), kf.v(), ALU.subtract)
    fw.ts(rr.v(), rr.v(), -PI, ALU.max, PI, ALU.min)
    fw.actf(cs[:, :, 0:16], rr.v(), AF.Sin)
    fw.barrier()
    AR.reset(m0)
    pass_mark = AR.mark()

    sctr = [0]

    def slab_srcs(l):
        def win(c0, n):
            return (W_IN[l][:, c0:c0 + n].re("(kc p) c -> p kc c", p=128), 8, n)
        lst = [win(C_VA, 512), win(C_KC, 512), win(C_KC + 512, 256), win(C_GC, 16), win(C_QC, 512),
               win(C_ZC, 512), win(C_UA, 512), win(C_ZA, 512)]
        lst += [win(C_GATE + 512 * i, 512) for i in range(6)]
        lst += [(W_BR[l, i].re("(kc p) c -> p kc c", p=128), 4, 1024) for i in range(3)]
        lst += [(W_OUT[l][:, 512 * i:512 * (i + 1)].re("(kc p) c -> p kc c", p=128), 8, 512) for i in range(2)]
        assert len(lst) == NSL
        return lst

    def convert_layer(l):
        AR.reset(pass_mark)
        stg = [A(f"cs{i}", [128, 2048], F32) for i in range(4)]
        ob = [A(f"co{i}", [128, 2048], BF16) for i in range(4)]
        kk = [0]

        def conv(dst_t, dst_fn, src_v, a, c):
            step = max(1, 2048 // c)
            for a0 in range(0, a, step):
                a1 = min(a, a0 + step)
                n = (a1 - a0) * c
                s_ = stg[kk[0] % 4]
                o_ = ob[kk[0] % 4]
                kk[0] += 1
                fw.dma(fw.sp, s_[:, 0:n].re("p (a c) -> p a c", a=a1 - a0), src_v[:, a0:a1, :])
                fw.copy(o_[:, 0:n], s_[:, 0:n])
                fw.dma(fw.act, dst_fn(a0 * c, n), o_[:, 0:n])
        fw.dma(fw.sp, cpp.v(), CPP[l])
        fw.dma(fw.sp, cbq.v(), CBC[l][:, BC_QG:BC_QG + 192])
        fw.dma(fw.sp, wgu.v(), W_GU[l])
        fw.dma(fw.sp, bgu.v(), B_GU[l])
        fw.cast_load(wuq.v(), W_UQ[l].re("(kc p) c -> p kc c", p=128), stg, sctr, eng=fw.dve)
        fw.cast_load(wukv.v().us(1), W_UKV[l].us(1), stg, sctr, eng=fw.dve)
        fw.cast_load(sgw.v(), SGWT[l].re("g j i -> j g i"), stg, sctr, eng=fw.dve)
        fw.memset(sgw[64:128, :, 0:64], 0.0)
        conv(W1, lambda o, n: W1[:, o:o + n], W_IN[l][:, C_CQ:C_CQ + 928].re("(kc p) c -> p kc c", p=128), 8, 928)
        for j, (src_v, a_, c_) in enumerate(slab_srcs(l)):
            conv(WS[j], (lambda o, n, j=j: WS[j][j, :, o:o + n]), src_v, a_, c_)
        fw.barrier()

    SCALE_A = float(96 ** -0.5)

    def interleave(gens):
        gens = list(gens)
        while gens:
            for g in list(gens):
                try:
                    next(g)
                except StopIteration:
                    gens.remove(g)

    def pass1(l, s):
        AR.reset(pass_mark)
        KTb = [A(f"KT{i}", [128, 8, 512], BF16) for i in range(nblk)]
        VCb = [A(f"VC{i}", [128, 4, 8, 66], BF16) for i in range(nblk)]
        win1 = A("win1", [128, 8, 928], BF16)
        xr = [A(f"xr{i}", [128, 1024], F32) for i in range(2)]
        hb = A("hb", [128, 1024], BF16)
        junk = A("junk", [128, 1024], BF16)
        hT = A("hT", [128, 8, 512], BF16)
        cqT = A("cqT", [128, 3, 512], BF16)
        zb2 = [A(f"zb{i}", [128, 4, 512], BF16) for i in range(2)]
        qT2 = [A(f"qT{i}", [128, 8, 512], BF16) for i in range(2)]
        qf = A("qf", [128, 768], F32)
        kr = A("kr", [128, 32], F32)
        qb = A("qb", [128, 8, 96], BF16)
        kb = A("kb", [128, 8, 96], BF16)
        knf = A("knf", [128, 8, 64], F32)
        krn = A("krn", [128, 8, 32], F32)
        rt = [A(f"rt{i}", [128, 8, 16], F32) for i in range(4)]
        pt = [A(f"pt{i}", [128, 512], BF16) for i in range(3)]
        ou = A("ou", [128, 512], F32)
        ytmp = A("ytmp", [128, 512], F32)
        ybT = A("ybT", [128, 4, 512], BF16)

        rden = ou
        sqf = junk
        fw.dma(fw.sp, win1.re("p a c -> p (a c)"), W1.v())
        for i in range(nblk):
            fw.memset(VCb[i][:, :, :, 64:65], 1.0, eng=fw.pool)

        def rope(dst_b, srcf, tix):
            cosb = cs[:, tix, 0:16].us(1).bc([128, 8, 16])
            sinb = cs[:, tix, 16:32].us(1).bc([128, 8, 16])
            x1 = srcf[:, :, 0:16]
            x2 = srcf[:, :, 16:32]
            fw.tt(rt[0].v(), x1, cosb, ALU.mult)
            fw.tt(rt[1].v(), x2, sinb, ALU.mult, eng=fw.pool)
            fw.tt(rt[2].v(), x1, sinb, ALU.mult)
            fw.tt(rt[3].v(), x2, cosb, ALU.mult, eng=fw.pool)
            fw.tt(dst_b[:, :, 0:16], rt[0].v(), rt[1].v(), ALU.subtract)
            fw.tt(dst_b[:, :, 16:32], rt[2].v(), rt[3].v(), ALU.add)

        psA = ring_alloc(psR[0:3])
        psC = ring_alloc(psR[3:4])

        def chain(b):
            zb = zb2[b % 2]
            qT = qT2[b % 2]
            KT = KTb[b]
            VC = VCb[b]
            for tl in range(4):
                r0 = b * 512 + tl * 128
                xt = xr[tl % 2]
                xin = X if l == 0 else Y[s][b * 4 + tl]
                fw.dma(fw.sp, xt.v(), xin[s, r0:r0 + 128, :])
                fw.actf(junk.v(), xt.v(), AF.Square, accum_out=st[:, 0:1])
                yield
                fw.ts(st[:, 1:2], st[:, 0:1], 1.0 / D_MODEL, ALU.mult, EPS, ALU.add)
                fw.rsqrt(st[:, 1:2], st[:, 1:2])
                yield
                fw.ts(hb.v(), xt.v(), st[:, 1:2], ALU.mult)
                yield
                for kc in range(8):
                    fw.transpose(psT[:, kc * 128:(kc + 1) * 128], hb[:, kc * 128:(kc + 1) * 128], ident_b.v(), inc=(kc == 7))
                yield
                fw.tt(hT[:, :, tl * 128:(tl + 1) * 128], psT.re("p (a b) -> p a b", a=8),
                      cpp[:, PP_NORM:PP_NORM + 8].us(2).bc([128, 8, 128]), ALU.mult)
                yield
            fw.dma(fw.sp, HT[s][b][s, b].re("p (a b) -> p a b", a=8), hT.v())
            for g in range(3):
                p = psC()
                for kc in range(8):
                    fw.mm(p.v(), win1[:, kc, g * 128:(g + 1) * 128], hT[:, kc, :], kc == 0, kc == 7)
                yield
                gcol = cpp[:, PP_CQG + g:PP_CQG + g + 1]
                fw.actf(cqT[:, g, :], p.v(), AF.Copy, scale=gcol)
                yield
            for g in range(4):
                p = psC()
                for kc in range(8):
                    fw.mm(p.v(), win1[:, kc, 416 + g * 128:416 + (g + 1) * 128], hT[:, kc, :], kc == 0, kc == 7)
                yield
                fw.actf(zb[:, g, :], p.v(), AF.Silu)
                yield
            for tl in range(4):
                tix = (s * nblk + b) * 4 + tl
                tc0 = tl * 128
                p = psC()
                for kc in range(8):
                    fw.mm(p[:, 0:416], hT[:, kc, tc0:tc0 + 128], win1[:, kc, 0:416], kc == 0, kc == 7)
                yield
                fw.actf(junk[:, 0:256], p[:, 0:256], AF.Square, accum_out=st[:, 2:3])
                fw.actf(junk[:, 256:384], p[:, 256:384], AF.Square, accum_out=st[:, 3:4])
                fw.copy(kr.v(), p[:, 384:416])
                yield
                fw.actf(junk[:, 384:416], kr.v(), AF.Square, accum_out=st[:, 4:5])
                fw.ts(st[:, 5:6], st[:, 2:3], EPS / 256.0, ALU.mult, EPS * EPS, ALU.add)
                yield
                fw.ts(st[:, 6:7], st[:, 3:4], 1.0 / 128.0, ALU.mult, EPS, ALU.add)
                fw.rsqrt(st[:, 6:7], st[:, 6:7])
                yield
                fw.tt(st[:, 7:8], st[:, 6:7], st[:, 6:7], ALU.mult)
                for kc in range(2):
                    fw.mm(psW[:, 0:512], cqT[:, kc, tc0:tc0 + 128], wuq[:, kc, 0:512], kc == 0, kc == 1)
                for kc in range(2):
                    fw.mm(psW[:, 512:768], cqT[:, kc, tc0:tc0 + 128], wuq[:, kc, 512:768], kc == 0, kc == 1)
                yield
                fw.actf(sqf[:, 0:768], psW[:, 0:768], AF.Square)
                yield
                fw.reduce(st[:, 8:16], sqf[:, 0:768].re("p (h d) -> p h d", h=8))
                yield
                fw.ts(st[:, 8:16], st[:, 8:16], 1.0 / 96.0, ALU.mult, st[:, 5:6], ALU.add)
                fw.rsqrt(st[:, 8:16], st[:, 8:16])
                yield
                fw.tt(qf.re("p (h d) -> p h d", h=8), psW[:, 0:768].re("p (h d) -> p h d", h=8),
                      st[:, 8:16].us(2).bc([128, 8, 96]), ALU.mult)
                yield
                q3 = qf.re("p (h d) -> p h d", h=8)
                fw.tt(q3, q3, cbq[:, 0:96].us(1).bc([128, 8, 96]), ALU.mult)
                yield
                fw.copy(qb[:, :, 0:64], q3[:, :, 0:64], eng=fw.pool)
                rope(qb[:, :, 64:96], q3[:, :, 64:96], tix)
                yield
                for h in range(8):
                    fw.transpose(psT[0:96, h * 128:(h + 1) * 128], qb[:, h, :], ident_b.v(), inc=(h == 7))
                yield
                fw.copy(qT[0:96, :, tc0:tc0 + 128], psT[0:96, :].re("p (a b) -> p a b", a=8), eng=fw.act)
                fw.mm(psW[:, 0:512], cqT[:, 2, tc0:tc0 + 128], wukv[:, 0:512], True, True)
                fw.mm(psW[:, 512:1024], cqT[:, 2, tc0:tc0 + 128], wukv[:, 512:1024], True, True)
                yield
                kv3 = psW.re("p (h d) -> p h d", h=8)
                fw.actf(sqf.v(), psW.v(), AF.Square)
                yield
                fw.reduce(st[:, 16:24], sqf.re("p (h d) -> p h d", h=8)[:, :, 0:64])
                yield
                fw.stt(st[:, 16:24], st[:, 16:24], st[:, 7:8], st[:, 4:5].bc([128, 8]), ALU.mult, ALU.add)
                fw.ts(st[:, 16:24], st[:, 16:24], 1.0 / 96.0, ALU.mult, EPS, ALU.add)
                yield
                fw.rsqrt(st[:, 16:24], st[:, 16:24])
                yield
                fw.ts(st[:, 24:32], st[:, 16:24], st[:, 6:7], ALU.mult)
                kg3 = cbq[:, 96:192].us(1).bc([128, 8, 96])
                fw.tt(knf.v(), kv3[:, :, 0:64], st[:, 24:32].us(2).bc([128, 8, 64]), ALU.mult)
                yield
                fw.tt(kb[:, :, 0:64], knf.v(), kg3[:, :, 0:64], ALU.mult, eng=fw.pool)
                fw.tt(krn.v(), kr.v().us(1).bc([128, 8, 32]), st[:, 16:24].us(2).bc([128, 8, 32]), ALU.mult)
                yield
                fw.tt(krn.v(), krn.v(), kg3[:, :, 64:96], ALU.mult)
                rope(kb[:, :, 64:96], krn.v(), tix)
                yield
                fw.ts(VC[:, tl, :, 0:64], kv3[:, :, 64:128], st[:, 6:7], ALU.mult)
                yield
                for h in range(8):
                    fw.transpose(psT[0:96, h * 128:(h + 1) * 128], kb[:, h, :], ident_b.v(), inc=(h == 7))
                yield
                fw.copy(KT[0:96, :, tc0:tc0 + 128], psT[0:96, :].re("p (a b) -> p a b", a=8), eng=fw.act)
                yield

        def attn(b):
            zb = zb2[b % 2]
            qT = qT2[b % 2]
            nk = 4 * b + 4
            pti = 0
            for h in range(8):
                po = psO

                def qk(kt):
                    j = kt - 4 * b
                    c0 = 128 * j if j > 0 else 0
                    p = psA()
                    fw.mm(p[:, c0:512], KTb[kt // 4][0:96, h, (kt % 4) * 128:(kt % 4 + 1) * 128], qT[0:96, h, c0:512], True, True)
                    return p, c0
                nxt = qk(0)
                for kt in range(nk):
                    p, c0 = nxt
                    if kt + 1 < nk:
                        nxt = qk(kt + 1)
                    ptile = pt[pti % 3]
                    pti += 1
                    fw.actf(ptile[:, c0:512], p[:, c0:512], AF.Exp, scale=SCALE_A)
                    if kt >= 4 * b:
                        fw.memset(ptile[64:128, c0:c0 + 64], 0.0, eng=fw.pool)
                    fw.mm(po[0:65, c0:512], VCb[kt // 4][:, kt % 4, h, 0:65], ptile[:, c0:512], kt == 0, kt == nk - 1)
                    yield
                fw.recip(rden[64:65, :], po[64:65, :])
                fw.copy(ou[0:64, :], po[0:64, :])
                yield
                pb = psA()
                fw.mm(pb[0:64, :], ones_f[64:65, 0:64], rden[64:65, :], True, True)
                yield
                hp = (h % 2) * 64
                fw.tt(ytmp[hp:hp + 64, :], ou[0:64, :], pb[0:64, :], ALU.mult)
                yield
                fw.tt(ybT[hp:hp + 64, h // 2, :], ytmp[hp:hp + 64, :], zb[hp:hp + 64, h // 2, :], ALU.mult, eng=fw.pool)
                yield
            fw.dma(fw.sp, YB[s][b][s, b].re("p (a b) -> p a b", a=4), ybT.v())

        interleave([chain(0)])
        for b in range(nblk):
            gens = [attn(b)]
            if b + 1 < nblk:
                gens.append(chain(b + 1))
            interleave(gens)
        fw.barrier()

    def pass2(l, s):
        AR.reset(pass_mark)
        NSLOT = 6
        ring = [A(f"ring{i}", [128, 4096], BF16) for i in range(NSLOT)]
        hT = A("hT2", [128, 8, 512], BF16)
        ybT = A("ybT2", [128, 4, 512], BF16)
        va = A("va", [128, 4, 512], BF16)
        vc = A("vc", [128, 4, 512], BF16)
        kct = A("kct", [128, 4, 256], F32)
        gT = A("gT", [16, 512], F32)
        qk = A("qk", [128, 4, 512], F32)
        ua = A("ua", [128, 4, 512], BF16)
        za = A("za", [128, 4, 512], BF16)
        zc = A("zc", [128, 4, 512], BF16)
        uz = A("uz", [128, 4, 512], BF16)
        gt = A("gt", [128, 24, 512], BF16)
        yaT = A("yaT", [128, 4, 512], BF16)
        ycT = A("ycT", [128, 4, 512], BF16)
        mg = A("mg", [128, 8, 512], BF16)
        xres = [A(f"xres{i}", [128, 1024], F32) for i in range(2)]
        kpad = A("kpad", [128, 4, 128], BF16)
        la = A("la", [128, 256], F32)
        eb = A("eb", [128, 2, 128], F32)
        enb = A("enb", [128, 2, 128], F32)
        qt = A("qt", [128, 2, 128], BF16)
        kt_ = A("kt_", [128, 2, 128], BF16)
        eD = A("eD", [128, 256], F32)
        attm = A("attm", [128, 4, 128], BF16)
        Sf = [A(f"Sf{i}", [128, 128], F32) for i in range(2)]
        Sb = [A(f"Sb{i}", [128, 128], BF16) for i in range(2)]
        gl = A("gl", [128, 512], F32)
        xc = A("xc", [128, 512], F32)
        t1 = A("t1", [128, 512], F32)
        t2 = A("t2", [128, 512], F32)
        t3 = A("t3", [128, 512], F32)
        sqb = A("sqb", [128, 512], BF16)
        junk = A("junk2", [128, 512], BF16)

        cbc = A("cb2", [128, 1280], F32)
        sgb = A("sgb", [128, 512], F32)
        fw.dma(fw.sp, cbc.v(), CBC[l][:, 0:1280])
        fw.dma(fw.sp, sgb.v(), CBC[l][:, BC_SGB:BC_SGB + 512])
        fw.memset(kpad.v(), 0.0, eng=fw.pool)
        for i in range(2):
            fw.memset(Sf[i].v(), 0.0, eng=fw.pool)
            fw.memset(Sb[i].v(), 0.0, eng=fw.pool)

        shapes = [(a_, c_) for (_, a_, c_) in slab_srcs(l)]
        sched = [(j, shapes[j][0], shapes[j][1]) for _ in range(nblk) for j in range(NSL)]
        state = {"issued": 0, "next": 0, "done": 0}

        def rel(n=1):
            state["done"] += n
            issue_until(state["done"] + NSLOT)

        def issue_until(n):
            while state["issued"] < min(n, len(sched)):
                i = state["issued"]
                j, a, c = sched[i]
                slot = ring[i % NSLOT]
                fw.dma(fw.sp, slot[:, 0:a * c], WS[j][j, :, 0:a * c])
                state["issued"] += 1

        def slab():
            i = state["next"]
            assert i < state["done"] + NSLOT
            issue_until(i + 1)
            state["next"] += 1
            _, a, c = sched[i]
            return ring[i % NSLOT][:, 0:a * c].re("p (a c) -> p a c", a=a)

        issue_until(NSLOT)

        psG = ring_alloc([psWa, psWb])

        def fm_group(w, c0, m=128, alloc=None):
            p = (alloc or ps)()
            for kc in range(8):
                fw.mm(p[0:m, :], w[:, kc, c0:c0 + m], hT[:, kc, :], kc == 0, kc == 7)
            return p

        for b in range(nblk):
            fw.dma(fw.sp, hT.v(), HT[s][b][s, b].re("p (a b) -> p a b", a=8))
            fw.dma(fw.sp, ybT.v(), YB[s][b][s, b].re("p (a b) -> p a b", a=4))
            w = slab()
            for tl in range(4):
                p = ps()
                for kc in range(8):
                    fw.mm(p.v(), hT[:, kc, tl * 128:(tl + 1) * 128], w[:, kc, :], kc == 0, kc == 7)
                fw.actf(gl.v(), p.v(), AF.Gelu, accum_out=st[:, 0:1])
                fw.ts(st[:, 1:2], st[:, 0:1], 1.0 / 512.0, ALU.mult)
                fw.ts(xc.v(), gl.v(), st[:, 1:2], ALU.subtract)
                fw.actf(junk.v(), xc.v(), AF.Square, accum_out=st[:, 2:3])
                fw.ts(st[:, 3:4], st[:, 2:3], 1.0 / 512.0, ALU.mult, EPS, ALU.add)
                fw.rsqrt(st[:, 3:4], st[:, 3:4])
                fw.stt(t1.v(), xc.v(), st[:, 3:4], cbc[:, BC_LNG:BC_LNG + 512], ALU.mult, ALU.mult)
                fw.tt(va[:, tl, :], t1.v(), cbc[:, BC_LNB:BC_LNB + 512], ALU.add, eng=fw.pool)
            rel()
            w = slab()
            for tl in range(4):
                p = ps()
                for kc in range(8):
                    fw.mm(p.v(), hT[:, kc, tl * 128:(tl + 1) * 128], w[:, kc, :], kc == 0, kc == 7)
                fw.copy(kct[:, tl, :], p[:, 0:256], eng=fw.act)
                fw.copy(vc[:, tl, 0:256], p[:, 256:512])
            rel()
            w = slab()
            for tl in range(4):
                p = ps()
                for kc in range(8):
                    fw.mm(p[:, 0:256], hT[:, kc, tl * 128:(tl + 1) * 128], w[:, kc, :], kc == 0, kc == 7)
                fw.copy(vc[:, tl, 256:512], p[:, 0:256], eng=fw.act)
            chk("p2.1")
            rel()
            w = slab()
            p = fm_group(w, 0, 16)
            fw.copy(gT.v(), p[0:16, :])
            rel()
            w = slab()
            for g in range(4):
                p = fm_group(w, g * 128)
                fw.copy(qk[:, g, :], p.v(), eng=fw.act)
            rel()
            w = slab()
            for g in range(4):
                p = fm_group(w, g * 128)
                fw.actf(zc[:, g, :], p.v(), AF.Silu)
            rel()
            w = slab()
            for g in range(4):
                p = fm_group(w, g * 128)
                fw.actf(ua[:, g, :], p.v(), AF.Gelu)
            rel()
            w = slab()
            for g in range(4):
                p = fm_group(w, g * 128)
                fw.actf(za[:, g, :], p.v(), AF.Silu)
            fw.tt(uz.v(), ua.v(), za.v(), ALU.mult, eng=fw.pool)
            def gates_gen():
                for i6 in range(6):
                    rel()
                    w = slab()
                    for g in range(4):
                        gi = i6 * 4 + g
                        p = fm_group(w, g * 128, alloc=psG)
                        yield
                        fw.actf(gt[:, gi, :], p.v(), AF.Sigmoid, bias=cpp[:, PP_BGATE + gi:PP_BGATE + gi + 1])
                        yield
            def gla_gen():
                for tl in range(4):
                    tc0 = tl * 128
                    p = ps()
                    for g in range(4):
                        fw.mm(p[:, g * 128:(g + 1) * 128], va[:, tl, g * 128:(g + 1) * 128], sgw[:, g, :], True, True, inc=(g == 3))
                    fw.tt(t1.v(), p.v(), sgb.v(), ALU.add)
                    yield
                    fw.tt(yaT[:, :, tc0:tc0 + 128], t1.re("p (g i) -> p g i", g=4), uz[:, :, tc0:tc0 + 128], ALU.mult, eng=fw.pool)
                    yield
                for tl in range(4):
                    tc0 = tl * 128
                    p = ps()
                    fw.mm(p[:, 0:256], gT[0:16, tc0:tc0 + 128], wgu[0:16, :], True, False, inc=False)
                    fw.mm(p[:, 0:256], ones_f[0:1, 0:128], bgu[0:1, :], False, True)
                    fw.actf(la.v(), p[:, 0:256], AF.Exp, scale=-1.0)
                    yield
                    fw.actf(la.v(), la.v(), AF.Ln, bias=1.0)
                    yield
                    fw.ts(la.v(), la.v(), -1.0 / 16.0, ALU.mult)
                    yield
                    pbT = ps()
                    for fc in range(2):
                        fw.mm(pbT[:, fc * 128:(fc + 1) * 128], la[:, fc * 128:(fc + 1) * 128], Tm_f, True, True, inc=(fc == 1))
                    pD = ps()
                    fw.mm(pD[:, 0:256], Tu_f, la.v(), True, True)
                    fw.actf(eb.re("p a b -> p (a b)"), pbT[:, 0:256], AF.Exp)
                    yield
                    fw.actf(enb.re("p a b -> p (a b)"), pbT[:, 0:256], AF.Exp, scale=-1.0)
                    yield
                    fw.actf(eD.v(), pD[:, 0:256], AF.Exp)
                    yield
                    fw.stt(qt.v(), qk[:, 0:2, tc0:tc0 + 128], 0.125, eb.v(), ALU.mult, ALU.mult)
                    yield
                    fw.tt(kt_.v(), qk[:, 2:4, tc0:tc0 + 128], enb.v(), ALU.mult, eng=fw.pool)
                    yield
                    for h in range(4):
                        o = (h % 2) * 64
                        fw.tt(kpad[:, h, o:o + 64], kct[:, tl, h * 64:(h + 1) * 64], eD[:, h * 64:(h + 1) * 64], ALU.mult,
                              eng=(fw.pool if h % 2 else fw.dve))
                    po = psO
                    for h in range(4):
                        fc = h // 2
                        o = (h % 2) * 64
                        pa = ps()
                        fw.mm(pa[:, 0:128], kt_[o:o + 64, fc, :], qt[o:o + 64, fc, :], True, True)
                        fw.tt(attm[:, h, :], pa[:, 0:128], Tm_b.v(), ALU.mult)
                        yield
                    for c in range(2):
                        r0 = c * 64
                        for h in range(4):
                            fc = h // 2
                            o = (h % 2) * 64
                            dst = po[:, h * 128 + r0:h * 128 + r0 + 64]
                            fw.mm(dst, vc[:, tl, h * 128:(h + 1) * 128], attm[:, h, r0:r0 + 64], True, False, inc=False)
                            fw.mm(dst, Sb[fc][o:o + 64, :], qt[o:o + 64, fc, r0:r0 + 64], False, True, inc=True)
                        for fc in range(2):
                            pk = ps()
                            for hh in range(2):
                                h = fc * 2 + hh
                                fw.mm(pk[:, 0:128], kpad[r0:r0 + 64, h, :], vc[r0:r0 + 64, tl, h * 128:(h + 1) * 128], hh == 0, hh == 1)
                            dec = eb[:, fc, r0 + 63:r0 + 64]
                            fw.stt(Sf[fc].v(), Sf[fc].v(), dec, pk[:, 0:128], ALU.mult, ALU.add)
                            yield
                            fw.copy(Sb[fc].v(), Sf[fc].v(), eng=fw.act)
                            yield
                    fw.actf(sqb.v(), po.v(), AF.Square)
                    yield
                    pss = ps()
                    fw.mm(pss.v(), ones_b.v(), sqb.v(), True, True)
                    fw.ts(t2.v(), pss.v(), 1.0 / 128.0, ALU.mult, EPS, ALU.add)
                    yield
                    fw.rsqrt(t2.v(), t2.v())
                    yield
                    fw.tt(t3.v(), po.v(), t2.v(), ALU.mult)
                    yield
                    fw.stt(ycT[:, :, tc0:tc0 + 128], t3.re("p (h i) -> p h i", h=4), cpp[:, PP_OG:PP_OG + 1],
                           zc[:, :, tc0:tc0 + 128], ALU.mult, ALU.mult)
            interleave([gates_gen(), gla_gen()])
            chk("gla")
            rel()
            wb = [slab() for _ in range(3)]
            ys = [yaT, ybT, ycT]
            for og in range(8):
                pp = []
                for i in range(3):
                    p = ps()
                    for kc in range(4):
                        fw.mm(p.v(), wb[i][:, kc, og * 128:(og + 1) * 128], ys[i][:, kc, :], kc == 0, kc == 3)
                    pp.append(p)
                fw.tt(t1.v(), pp[0].v(), gt[:, og, :], ALU.mult)
                fw.tt(t2.v(), pp[1].v(), gt[:, 8 + og, :], ALU.mult)
                fw.tt(t3.v(), pp[2].v(), gt[:, 16 + og, :], ALU.mult)
                fw.tt(t1.v(), t1.v(), t2.v(), ALU.add, eng=fw.pool)
                fw.tt(mg[:, og, :], t1.v(), t3.v(), ALU.add, eng=fw.pool)
            chk("p2.4")
            rel(3)
            wo = [slab() for _ in range(2)]
            for tl in range(4):
                r0 = b * 512 + tl * 128
                xt = xres[tl % 2]
                xin = X if l == 0 else Y[s][b * 4 + tl]
                fw.dma(fw.sp, xt.v(), xin[s, r0:r0 + 128, :])
                for hf in range(2):
                    p = ps()
                    for kc in range(8):
                        fw.mm(p.v(), mg[:, kc, tl * 128:(tl + 1) * 128], wo[hf][:, kc, :], kc == 0, kc == 7)
                    fw.tt(xt[:, hf * 512:(hf + 1) * 512], xt[:, hf * 512:(hf + 1) * 512], p.v(), ALU.add)
                fw.dma(fw.sp, Y[s][b * 4 + tl][s, r0:r0 + 128, :], xt.v())
            rel(2)
        fw.barrier()

    try:
        chk("setup")
        for l in range(depth):
            convert_layer(l)
            chk("consts")
            for s in range(nseq):
                pass1(l, s)
                chk("pass1")
                pass2(l, s)
    except _Stop:
        pass
    fw.barrier()
    return nc, fw


def _consts():
    j = np.arange(128)[:, None]
    i = np.arange(128)[None, :]
    same = (j // 64) == (i // 64)
    Tm = (same & (j <= i)).astype(np.float32)
    Tu = (same & (j > i)).astype(np.float32)
    half = 16
    inv = (np.float32(1.0) / np.power(np.float32(10000.0),
                                      np.arange(half, dtype=np.float32) * np.float32(2.0) / np.float32(32))).astype(np.float32)
    cst = np.concatenate([np.eye(128, dtype=np.float32), Tm, Tu, np.broadcast_to(inv[None, :], (128, 16))], axis=1)
    return np.ascontiguousarray(cst)


def _pack(inputs, depth):
    f = lambda k: np.asarray(inputs[k], dtype=np.float32)
    cpp = np.zeros((depth, 128, NPP), np.float32)
    cbc = np.zeros((depth, 128, NBC), np.float32)
    for l in range(depth):
        cpp[l, :, PP_NORM:PP_NORM + 8] = f("norm_g")[l].reshape(8, 128).T
        cpp[l, :, PP_CQG:PP_CQG + 2] = f("mla_cq_g")[l].reshape(2, 128).T
        cpp[l, :, PP_CKVG] = f("mla_ckv_g")[l]
        cpp[l, :, PP_BGATE:PP_BGATE + 24] = f("b_gate")[l].reshape(24, 128).T
        cpp[l, :, PP_OG] = f("gla_o_g")[l]
        cbc[l, :, BC_LNG:BC_LNG + 512] = f("sg_ln_g")[l][None, :]
        cbc[l, :, BC_LNB:BC_LNB + 512] = f("sg_ln_b")[l][None, :]
        cbc[l, :, BC_BGU:BC_BGU + 256] = f("gla_b_gate")[l][None, :]
        cbc[l, :, BC_QG:BC_QG + 96] = f("mla_q_g")[l][None, :]
        cbc[l, :, BC_KG:BC_KG + 96] = f("mla_k_g")[l][None, :]
        cbc[l, :, BC_SGB:BC_SGB + 512] = f("sg_b")[l].reshape(512)[None, :]
    return cpp, cbc


_CACHE = {}


def run(inputs, n_cores, nseq, nblk, depth):
    key = (nseq, nblk, depth)
    if key not in _CACHE:
        _CACHE[key] = build(nseq, nblk, depth)
    nc, fw = _CACHE[key]
    S = nblk * 512
    x = np.asarray(inputs["x"], np.float32)
    pos = np.asarray(inputs["positions"], np.int32)
    cpp, cbc = _pack(inputs, depth)
    cst = _consts()
    f = lambda k: np.ascontiguousarray(np.asarray(inputs[k], np.float32)[:depth])
    shared = {
        "w_in": f("w_in"), "w_uq": f("mla_w_uq"), "w_ukv": f("mla_w_ukv"), "w_gu": f("gla_w_gate"),
        "b_gu": f("gla_b_gate").reshape(depth, 1, 256),
        "sg_wT": np.ascontiguousarray(f("sg_w").transpose(0, 1, 3, 2)),
        "w_branch": f("w_branch"), "w_out": f("w_out"), "cpp": cpp, "cbc": cbc, "cst": cst,
    }
    in_maps = []
    for c in range(n_cores):
        xs = np.ascontiguousarray(x[c * nseq:(c + 1) * nseq, :S])
        ps_ = pos[c * nseq:(c + 1) * nseq, :S]
        posT = np.ascontiguousarray(ps_.reshape(nseq * nblk * 4, 128).T)
        m = dict(shared)
        m["x"] = xs
        m["posT"] = posT
        in_maps.append(m)
    res = run_bass_kernel_spmd(nc, in_maps, core_ids=list(range(n_cores)))
    return np.concatenate([r["y"] for r in res.results], axis=0)


def kernel(**inputs):
    return run(inputs, N_CORES, 2, 8, 4)
```

```python
import numpy as np
import ml_dtypes
import concourse.bass as bass
import concourse.mybir as mybir
from concourse.bass_utils import run_bass_kernel_spmd

F32 = mybir.dt.float32
BF16 = mybir.dt.bfloat16
I32 = mybir.dt.int32
AF = mybir.ActivationFunctionType
ALU = mybir.AluOpType
AX = mybir.AxisListType

D_MODEL = 1024
IN_COLS = 7088
EPS = 1e-6
N_CORES = 8
PI = 3.141592


class Sem:
    def __init__(self, handle, is_dma):
        self.h = handle
        self.issued = 0
        self.is_dma = is_dma


class V:
    def __init__(self, t, ap):
        self.t = t
        self.ap = ap

    def __getitem__(self, idx):
        return V(self.t, self.ap[idx])

    def re(self, s, **kw):
        return V(self.t, self.ap.rearrange(s, **kw))

    def bc(self, shape):
        return V(self.t, self.ap.to_broadcast(list(shape)))

    def us(self, axis):
        return V(self.t, self.ap.unsqueeze(axis))


class T:
    def __init__(self, ap, name=""):
        self.ap = ap
        self.name = name
        self.w = None
        self.r = {}
        self.dsem = None
        self.is_dram = False

    def __getitem__(self, idx):
        return V(self, self.ap[idx])

    def v(self):
        return V(self, self.ap)

    def re(self, s, **kw):
        return V(self, self.ap.rearrange(s, **kw))


class Eng:
    def __init__(self, h, sem, name, self_sync=True):
        self.h = h
        self.sem = sem
        self.name = name
        self.waited = {}
        self.self_sync = self_sync


class FW:
    def __init__(self, nc):
        self.nc = nc
        self.n_sems = 0
        self.all_sems = []
        mk = lambda h, n, ss=True: Eng(h, self.new_sem(n, False), n, ss)
        self.pe = mk(nc.tensor, "pe", False)
        self.act = mk(nc.scalar, "act")
        self.dve = mk(nc.vector, "dve")
        self.pool = mk(nc.gpsimd, "pool")
        self.sp = mk(nc.sync, "sp")
        self.engs = [self.pe, self.act, self.dve, self.pool, self.sp]
        self.n_inst = 0
        self.n_wait = 0
        self.dsems = {}

    def new_sem(self, name, is_dma):
        self.n_sems += 1
        s = Sem(self.nc.alloc_semaphore(name), is_dma)
        self.all_sems.append(s)
        return s

    def dram(self, ap, name=""):
        t = T(ap, name)
        t.is_dram = True
        return t

    def _wait(self, eng, rd, wr):
        deps = {}

        def add(sv):
            if sv is not None:
                s, v = sv
                if deps.get(s, 0) < v:
                    deps[s] = v
        for t in rd:
            add(t.w)
        for t in wr:
            add(t.w)
            for s, v in t.r.items():
                add((s, v))
        for s, v in deps.items():
            if s.is_dma:
                v = s.issued
            if s is eng.sem and not eng.self_sync:
                continue
            if eng.waited.get(s, 0) >= v:
                continue
            eng.h.wait_ge(s.h, v)
            eng.waited[s] = v
            self.n_wait += 1

    def op(self, eng, fn, rd, wr, inc=True):
        rd = [x.t for x in rd if isinstance(x, V)]
        wr = [x.t for x in wr if isinstance(x, V)]
        self._wait(eng, rd, wr)
        ins = fn()
        self.n_inst += 1
        if inc:
            eng.sem.issued += 1
            ins.then_inc(eng.sem.h, 1)
            v = eng.sem.issued
        else:
            v = eng.sem.issued + 1
        for t in rd:
            t.r[eng.sem] = v
        for t in wr:
            t.w = (eng.sem, v)
            t.r = {}
        return ins

    def dma(self, q, out, in_, sem_t=None, **kw):
        if sem_t is None:
            sem_t = out.t if not out.t.is_dram else (in_.t if not in_.t.is_dram else out.t)
        if sem_t.dsem is None:
            if sem_t.name not in self.dsems:
                self.dsems[sem_t.name] = self.new_sem("d_" + sem_t.name, True)
            sem_t.dsem = self.dsems[sem_t.name]
        s = sem_t.dsem
        self._wait(q, [in_.t], [out.t])
        ins = q.h.dma_start(out=out.ap, in_=in_.ap, **kw)
        s.issued += 16
        ins.then_inc(s.h, 16)
        self.n_inst += 1
        in_.t.r[s] = s.issued
        out.t.w = (s, s.issued)
        out.t.r = {}
        return ins

    def dma_split(self, q, out, in_):
        for a in range(out.ap.shape[1]):
            self.dma(q, out[:, a, :], in_[:, a, :])

    def cast_load(self, dst, src, stages, ctr, eng=None):
        e = eng if eng is not None else self.pool
        np_, na, ncol = dst.ap.shape
        for a in range(na):
            stg = stages[ctr[0] % len(stages)]
            ctr[0] += 1
            self.dma(self.sp, stg[0:np_, 0:ncol], src[:, a, :])
            self.copy(dst[:, a, :], stg[0:np_, 0:ncol], eng=e)

    def wait_all(self, eng, tiles):
        self._wait(eng, list(tiles), [])

    def barrier(self):
        for e in self.engs:
            for s in self.all_sems:
                if s is e.sem or s.issued == 0:
                    continue
                if e.waited.get(s, 0) >= s.issued:
                    continue
                e.h.wait_ge(s.h, s.issued)
                e.waited[s] = s.issued
                self.n_wait += 1

    def mm(self, out, lhsT, rhs, start, stop, inc=None):
        if inc is None:
            inc = stop
        return self.op(self.pe, lambda: self.nc.tensor.matmul(out.ap, lhsT.ap, rhs.ap, start=start, stop=stop),
                       [lhsT, rhs], [out], inc=inc)

    def transpose(self, out, in_, ident, inc=True):
        return self.op(self.pe, lambda: self.nc.tensor.transpose(out.ap, in_.ap, ident.ap),
                       [in_, ident], [out], inc=inc)

    def actf(self, out, in_, func, bias=None, scale=None, accum_out=None):
        kw = {}
        rd = [in_]
        wr = [out]
        if bias is not None:
            kw["bias"] = bias.ap if isinstance(bias, V) else bias
            rd.append(bias)
        if scale is not None:
            kw["scale"] = scale.ap if isinstance(scale, V) else scale
            rd.append(scale)
        if accum_out is not None:
            kw["accum_out"] = accum_out.ap
            wr.append(accum_out)
        return self.op(self.act, lambda: self.nc.scalar.activation(out=out.ap, in_=in_.ap, func=func, **kw), rd, wr)

    def _veng(self, eng):
        return eng if eng is not None else self.dve

    def tt(self, out, in0, in1, op, eng=None):
        e = self._veng(eng)
        return self.op(e, lambda: e.h.tensor_tensor(out=out.ap, in0=in0.ap, in1=in1.ap, op=op), [in0, in1], [out])

    def ts(self, out, in0, s1, op0, s2=None, op1=None, eng=None):
        e = self._veng(eng)
        a1 = s1.ap if isinstance(s1, V) else s1
        a2 = s2.ap if isinstance(s2, V) else s2
        kw = {}
        if op1 is not None:
            kw["op1"] = op1
        return self.op(e, lambda: e.h.tensor_scalar(out=out.ap, in0=in0.ap, scalar1=a1, scalar2=a2, op0=op0, **kw),
                       [in0, s1, s2], [out])

    def stt(self, out, in0, scalar, in1, op0, op1, eng=None):
        e = self._veng(eng)
        a = scalar.ap if isinstance(scalar, V) else scalar
        return self.op(e, lambda: e.h.scalar_tensor_tensor(out=out.ap, in0=in0.ap, scalar=a, in1=in1.ap, op0=op0, op1=op1),
                       [in0, scalar, in1], [out])

    def copy(self, out, in_, eng=None):
        e = self._veng(eng)
        if e is self.act:
            return self.op(e, lambda: self.nc.scalar.copy(out=out.ap, in_=in_.ap), [in_], [out])
        return self.op(e, lambda: e.h.tensor_copy(out=out.ap, in_=in_.ap), [in_], [out])

    def memset(self, out, val, eng=None):
        e = self._veng(eng)
        return self.op(e, lambda: e.h.memset(out.ap, val), [], [out])

    def reduce(self, out, in_, op=ALU.add, axis=AX.X, eng=None):
        e = self._veng(eng)
        return self.op(e, lambda: e.h.tensor_reduce(out=out.ap, in_=in_.ap, axis=axis, op=op), [in_], [out])

    def recip(self, out, in_):
        return self.op(self.dve, lambda: self.nc.vector.reciprocal(out=out.ap, in_=in_.ap), [in_], [out])

    def rsqrt(self, out, in_):
        self.actf(out, in_, AF.Ln)
        self.actf(out, out, AF.Exp, scale=-0.5)


class Arena:
    def __init__(self, ap, nfloats):
        self.ap = ap
        self.n = nfloats
        self.off = 0
        self.marks = []

    def alloc(self, name, shape, dt):
        esz = 4 if dt in (F32, I32) else 2
        free = int(np.prod(shape[1:]))
        nby = free * esz
        nfl = (nby + 31) // 32 * 8
        assert self.off + nfl <= self.n, f"arena overflow at {name}: {self.off + nfl} > {self.n}"
        a = self.ap[:, self.off:self.off + nfl]
        self.off += nfl
        if dt != F32:
            a = a.bitcast(dt)
        a = a[0:shape[0], 0:free]
        if len(shape) == 3:
            a = a.rearrange("p (a b) -> p a b", a=shape[1])
        elif len(shape) == 4:
            a = a.rearrange("p (a b c) -> p a b c", a=shape[1], b=shape[2])
        return T(a, name)

    def mark(self):
        return self.off

    def reset(self, m):
        self.off = m


PP_NORM, PP_CQG, PP_CKVG, PP_BGATE, PP_OG, NPP = 0, 8, 10, 11, 35, 36
BC_LNG, BC_LNB, BC_BGU, BC_QG, BC_KG, BC_SGB, NBC = 0, 512, 1024, 1280, 1376, 1472, 1984

C_UA, C_VA, C_ZA, C_CQ, C_CKV, C_KR, C_ZB, C_QC, C_KC, C_VC, C_GC, C_ZC, C_GATE = (
    0, 512, 1024, 1536, 1792, 1920, 1952, 2464, 2720, 2976, 3488, 3504, 4016)


STOP = None


class _Stop(Exception):
    pass


def chk(name):
    if STOP == name:
        raise _Stop()


def build(nseq, nblk, depth):
    S = nblk * 512
    NT = nseq * nblk * 4
    nc = bass.Bass("TRN2", target_bir_lowering=False)
    fw = FW(nc)

    def din(name, shape, dt=F32):
        return fw.dram(nc.dram_tensor(name, list(shape), dt, kind="ExternalInput").ap(), name)

    X = din("x", [nseq, S, D_MODEL])
    POS = din("posT", [128, NT], I32)
    W_IN = din("w_in", [depth, D_MODEL, IN_COLS])
    W_UQ = din("w_uq", [depth, 256, 768])
    W_UKV = din("w_ukv", [depth, 128, 1024])
    W_GU = din("w_gu", [depth, 16, 256])
    B_GU = din("b_gu", [depth, 1, 256])
    SGWT = din("sg_wT", [depth, 4, 128, 128])
    W_BR = din("w_branch", [depth, 3, 512, 1024])
    W_OUT = din("w_out", [depth, D_MODEL, D_MODEL])
    CPP = din("cpp", [depth, 128, NPP])
    CBC = din("cbc", [depth, 128, NBC])
    CST = din("cst", [128, 128 * 3 + 16])
    Yh = nc.dram_tensor("y", [nseq, S, D_MODEL], F32, kind="ExternalOutput").ap()
    Y = [[fw.dram(Yh, f"y{s}_{t}") for t in range(nblk * 4)] for s in range(nseq)]
    HTh = nc.dram_tensor("ht_scr", [nseq, nblk, 128, 8 * 512], BF16).ap()
    HT = [[fw.dram(HTh, f"ht{s}_{b}") for b in range(nblk)] for s in range(nseq)]
    YBh = nc.dram_tensor("yb_scr", [nseq, nblk, 128, 4 * 512], BF16).ap()
    YB = [[fw.dram(YBh, f"yb{s}_{b}") for b in range(nblk)] for s in range(nseq)]

    NSL = 19
    WSh = nc.dram_tensor("ws_scr", [NSL, 128, 4096], BF16).ap()
    WS = [fw.dram(WSh, f"ws{i}") for i in range(NSL)]
    W1h = nc.dram_tensor("w1_scr", [128, 7424], BF16).ap()
    W1 = fw.dram(W1h, "w1")
    NFL = 52400
    arena_h = nc.alloc_sbuf_tensor("arena", [128, NFL], F32)
    AR = Arena(arena_h.ap(), NFL)
    A = AR.alloc

    psW = T(nc.alloc_psum_tensor("psW", [128, 1024], F32).ap(), "psW")
    psT = T(nc.alloc_psum_tensor("psT", [128, 1024], BF16).ap(), "psT")
    psO = T(nc.alloc_psum_tensor("psO", [128, 512], F32).ap(), "psO")
    NPS = 4
    psR = [T(nc.alloc_psum_tensor(f"psR{i}", [128, 512], F32).ap(), f"psR{i}") for i in range(NPS)]
    def ring_alloc(tiles):
        ctr = [0]

        def f():
            t = tiles[ctr[0] % len(tiles)]
            ctr[0] += 1
            return t
        return f
    ps = ring_alloc(psR)
    psWa = T(psW.ap[:, 0:512], "psWa")
    psWb = T(psW.ap[:, 512:1024], "psWb")

    cst = A("cst", [128, 400], F32)
    ident_b = A("ident_b", [128, 128], BF16)
    Tm_b = A("Tm_b", [128, 128], BF16)
    ones_b = A("ones_b", [128, 128], BF16)
    ones_f = A("ones_f", [128, 128], F32)
    cs = A("cs", [128, NT, 32], F32)
    cpp = A("cpp", [128, NPP], F32)
    cbq = A("cbq", [128, 192], F32)
    wuq = A("wuq", [128, 2, 768], BF16)
    wukv = A("wukv", [128, 1024], BF16)
    sgw = A("sgw", [128, 4, 128], BF16)
    wgu = A("wgu", [16, 256], F32)
    bgu = A("bgu", [1, 256], F32)
    st = A("st", [128, 64], F32)
    Tm_f = cst[:, 128:256]
    Tu_f = cst[:, 256:384]
    invf = cst[:, 384:400]

    fw.dma(fw.sp, cst.v(), CST.v())
    fw.copy(ident_b.v(), cst[:, 0:128])
    fw.copy(Tm_b.v(), Tm_f)
    fw.memset(ones_b.v(), 1.0)
    fw.memset(ones_f.v(), 1.0)
    m0 = AR.mark()
    posi = A("posi", [128, NT], I32)
    posf = A("posf", [128, NT], F32)
    ang = A("ang", [128, NT, 16], F32)
    kf = A("kf", [128, NT, 16], F32)
    ki = A("ki", [128, NT, 16], I32)
    rr = A("rr", [128, NT, 16], F32)
    fw.dma(fw.sp, posi.v(), POS.v())
    fw.copy(posf.v(), posi.v())
    fw.tt(ang.v(), posf.v().us(2).bc([128, NT, 16]), invf.us(1).bc([128, NT, 16]), ALU.mult)
    fw.ts(kf.v(), ang.v(), float(1.0 / (2 * np.pi)), ALU.mult)
    fw.copy(ki.v(), kf.v())
    fw.copy(kf.v(), ki.v())
    C1 = 6.28125
    C2 = float(2 * np.pi - 6.28125)
    fw.stt(rr.v(), kf.v(), -C1, ang.v(), ALU.mult, ALU.add)
    fw.stt(rr.v(), kf.v(), -C2, rr.v(), ALU.mult, ALU.add)
    fw.ts(ang.v(), rr.v(), -PI, ALU.max, PI, ALU.min)
    fw.actf(cs[:, :, 16:32], ang.v(), AF.Sin)
    fw.ts(rr.v(), rr.v(), float(np.pi / 2), ALU.add)
    fw.ts(kf.v(), rr.v(), float(np.pi), ALU.is_gt, float(2 * np.pi), ALU.mult)
    fw.tt(rr.v(), rr.v(), kf.v(), ALU.subtract)
    fw.ts(rr.v(), rr.v(), -PI, ALU.max, PI, ALU.min)
    fw.actf(cs[:, :, 0:16], rr.v(), AF.Sin)
    fw.barrier()
    AR.reset(m0)
    pass_mark = AR.mark()

    sctr = [0]

    def slab_srcs(l):
        def win(c0, n):
            return (W_IN[l][:, c0:c0 + n].re("(kc p) c -> p kc c", p=128), 8, n)
        lst = [win(C_VA, 512), win(C_KC, 512), win(C_KC + 512, 256), win(C_GC, 16), win(C_QC, 512),
               win(C_ZC, 512), win(C_UA, 512), win(C_ZA, 512)]
        lst += [win(C_GATE + 512 * i, 512) for i in range(6)]
        lst += [(W_BR[l, i].re("(kc p) c -> p kc c", p=128), 4, 1024) for i in range(3)]
        lst += [(W_OUT[l][:, 512 * i:512 * (i + 1)].re("(kc p) c -> p kc c", p=128), 8, 512) for i in range(2)]
        assert len(lst) == NSL
        return lst

    def convert_layer(l):
        AR.reset(pass_mark)
        stg = [A(f"cs{i}", [128, 2048], F32) for i in range(4)]
        ob = [A(f"co{i}", [128, 2048], BF16) for i in range(4)]
        kk = [0]

        def conv(dst_t, dst_fn, src_v, a, c):
            step = max(1, 2048 // c)
            for a0 in range(0, a, step):
                a1 = min(a, a0 + step)
                n = (a1 - a0) * c
                s_ = stg[kk[0] % 4]
                o_ = ob[kk[0] % 4]
                kk[0] += 1
                fw.dma(fw.sp, s_[:, 0:n].re("p (a c) -> p a c", a=a1 - a0), src_v[:, a0:a1, :])
                fw.copy(o_[:, 0:n], s_[:, 0:n])
                fw.dma(fw.act, dst_fn(a0 * c, n), o_[:, 0:n])
        fw.dma(fw.sp, cpp.v(), CPP[l])
        fw.dma(fw.sp, cbq.v(), CBC[l][:, BC_QG:BC_QG + 192])
        fw.dma(fw.sp, wgu.v(), W_GU[l])
        fw.dma(fw.sp, bgu.v(), B_GU[l])
        fw.cast_load(wuq.v(), W_UQ[l].re("(kc p) c -> p kc c", p=128), stg, sctr, eng=fw.dve)
        fw.cast_load(wukv.v().us(1), W_UKV[l].us(1), stg, sctr, eng=fw.dve)
        fw.cast_load(sgw.v(), SGWT[l].re("g j i -> j g i"), stg, sctr, eng=fw.dve)
        fw.memset(sgw[64:128, :, 0:64], 0.0)
        conv(W1, lambda o, n: W1[:, o:o + n], W_IN[l][:, C_CQ:C_CQ + 928].re("(kc p) c -> p kc c", p=128), 8, 928)
        for j, (src_v, a_, c_) in enumerate(slab_srcs(l)):
            conv(WS[j], (lambda o, n, j=j: WS[j][j, :, o:o + n]), src_v, a_, c_)
        fw.barrier()

    SCALE_A = float(96 ** -0.5)

    def interleave(gens):
        gens = list(gens)
        while gens:
            for g in list(gens):
                try:
                    next(g)
                except StopIteration:
                    gens.remove(g)

    def pass1(l, s):
        AR.reset(pass_mark)
        KTb = [A(f"KT{i}", [128, 8, 512], BF16) for i in range(nblk)]
        VCb = [A(f"VC{i}", [128, 4, 8, 66], BF16) for i in range(nblk)]
        win1 = A("win1", [128, 8, 928], BF16)
        xr = [A(f"xr{i}", [128, 1024], F32) for i in range(3)]
        hb = A("hb", [128, 1024], BF16)
        junk = A("junk", [128, 1024], BF16)
        hT = A("hT", [128, 8, 512], BF16)
        cqT = A("cqT", [128, 3, 512], BF16)
        zb2 = [A(f"zb{i}", [128, 4, 512], BF16) for i in range(2)]
        qT2 = [A(f"qT{i}", [128, 8, 512], BF16) for i in range(2)]
        qf = A("qf", [128, 768], F32)
        kr = A("kr", [128, 32], F32)
        qb = A("qb", [128, 8, 96], BF16)
        kb = A("kb", [128, 8, 96], BF16)
        knf = A("knf", [128, 8, 64], F32)
        krn = A("krn", [128, 8, 32], F32)
        rt = [A(f"rt{i}", [128, 8, 16], F32) for i in range(4)]
        pt = [A(f"pt{i}", [128, 512], BF16) for i in range(3)]
        ou = A("ou", [128, 512], F32)
        ytmp = A("ytmp", [128, 512], F32)
        ybT = A("ybT", [128, 4, 512], BF16)

        rden = ou
        sqf = junk
        fw.dma(fw.sp, win1.re("p a c -> p (a c)"), W1.v())
        for i in range(nblk):
            fw.memset(VCb[i][:, :, :, 64:65], 1.0, eng=fw.pool)

        def rope(dst_b, srcf, tix):
            cosb = cs[:, tix, 0:16].us(1).bc([128, 8, 16])
            sinb = cs[:, tix, 16:32].us(1).bc([128, 8, 16])
            x1 = srcf[:, :, 0:16]
            x2 = srcf[:, :, 16:32]
            fw.tt(rt[0].v(), x1, cosb, ALU.mult)
            fw.tt(rt[1].v(), x2, sinb, ALU.mult, eng=fw.pool)
            fw.tt(rt[2].v(), x1, sinb, ALU.mult)
            fw.tt(rt[3].v(), x2, cosb, ALU.mult, eng=fw.pool)
            fw.tt(dst_b[:, :, 0:16], rt[0].v(), rt[1].v(), ALU.subtract)
            fw.tt(dst_b[:, :, 16:32], rt[2].v(), rt[3].v(), ALU.add)

        psA = ring_alloc(psR[0:3])
        psC = ring_alloc(psR[3:4])

        def chain(b):
            zb = zb2[b % 2]
            qT = qT2[b % 2]
            KT = KTb[b]
            VC = VCb[b]
            def xload(tl):
                r0 = b * 512 + tl * 128
                xin = X if l == 0 else Y[s][b * 4 + tl]
                fw.dma(fw.sp, xr[tl % 3].v(), xin[s, r0:r0 + 128, :])
            for tl in range(3):
                xload(tl)
            for tl in range(4):
                xt = xr[tl % 3]
                fw.actf(junk.v(), xt.v(), AF.Square, accum_out=st[:, 0:1])
                yield
                fw.ts(st[:, 1:2], st[:, 0:1], 1.0 / D_MODEL, ALU.mult, EPS, ALU.add)
                fw.rsqrt(st[:, 1:2], st[:, 1:2])
                yield
                fw.ts(hb.v(), xt.v(), st[:, 1:2], ALU.mult)
                if tl == 0:
                    xload(3)
                yield
                for kc in range(8):
                    fw.transpose(psT[:, kc * 128:(kc + 1) * 128], hb[:, kc * 128:(kc + 1) * 128], ident_b.v(), inc=(kc == 7))
                yield
                fw.tt(hT[:, :, tl * 128:(tl + 1) * 128], psT.re("p (a b) -> p a b", a=8),
                      cpp[:, PP_NORM:PP_NORM + 8].us(2).bc([128, 8, 128]), ALU.mult)
                yield
            fw.dma(fw.sp, HT[s][b][s, b].re("p (a b) -> p a b", a=8), hT.v())
            for g in range(3):
                p = psC()
                for kc in range(8):
                    fw.mm(p.v(), win1[:, kc, g * 128:(g + 1) * 128], hT[:, kc, :], kc == 0, kc == 7)
                yield
                gcol = cpp[:, PP_CQG + g:PP_CQG + g + 1]
                fw.actf(cqT[:, g, :], p.v(), AF.Copy, scale=gcol)
                yield
            for g in range(4):
                p = psC()
                for kc in range(8):
                    fw.mm(p.v(), win1[:, kc, 416 + g * 128:416 + (g + 1) * 128], hT[:, kc, :], kc == 0, kc == 7)
                yield
                fw.actf(zb[:, g, :], p.v(), AF.Silu)
                yield
            for tl in range(4):
                tix = (s * nblk + b) * 4 + tl
                tc0 = tl * 128
                p = psC()
                for kc in range(8):
                    fw.mm(p[:, 0:416], hT[:, kc, tc0:tc0 + 128], win1[:, kc, 0:416], kc == 0, kc == 7)
                yield
                fw.actf(junk[:, 0:256], p[:, 0:256], AF.Square, accum_out=st[:, 2:3])
                fw.actf(junk[:, 256:384], p[:, 256:384], AF.Square, accum_out=st[:, 3:4])
                fw.copy(kr.v(), p[:, 384:416])
                yield
                fw.actf(junk[:, 384:416], kr.v(), AF.Square, accum_out=st[:, 4:5])
                fw.ts(st[:, 5:6], st[:, 2:3], EPS / 256.0, ALU.mult, EPS * EPS, ALU.add)
                yield
                fw.ts(st[:, 6:7], st[:, 3:4], 1.0 / 128.0, ALU.mult, EPS, ALU.add)
                fw.rsqrt(st[:, 6:7], st[:, 6:7])
                yield
                fw.tt(st[:, 7:8], st[:, 6:7], st[:, 6:7], ALU.mult)
                for kc in range(2):
                    fw.mm(psW[:, 0:512], cqT[:, kc, tc0:tc0 + 128], wuq[:, kc, 0:512], kc == 0, kc == 1)
                for kc in range(2):
                    fw.mm(psW[:, 512:768], cqT[:, kc, tc0:tc0 + 128], wuq[:, kc, 512:768], kc == 0, kc == 1)
                yield
                fw.actf(sqf[:, 0:768], psW[:, 0:768], AF.Square)
                yield
                fw.reduce(st[:, 8:16], sqf[:, 0:768].re("p (h d) -> p h d", h=8))
                yield
                fw.ts(st[:, 8:16], st[:, 8:16], 1.0 / 96.0, ALU.mult, st[:, 5:6], ALU.add)
                fw.rsqrt(st[:, 8:16], st[:, 8:16])
                yield
                fw.tt(qf.re("p (h d) -> p h d", h=8), psW[:, 0:768].re("p (h d) -> p h d", h=8),
                      st[:, 8:16].us(2).bc([128, 8, 96]), ALU.mult)
                yield
                q3 = qf.re("p (h d) -> p h d", h=8)
                fw.tt(q3, q3, cbq[:, 0:96].us(1).bc([128, 8, 96]), ALU.mult)
                yield
                fw.copy(qb[:, :, 0:64], q3[:, :, 0:64], eng=fw.pool)
                rope(qb[:, :, 64:96], q3[:, :, 64:96], tix)
                yield
                for h in range(8):
                    fw.transpose(psT[0:96, h * 128:(h + 1) * 128], qb[:, h, :], ident_b.v(), inc=(h == 7))
                yield
                fw.copy(qT[0:96, :, tc0:tc0 + 128], psT[0:96, :].re("p (a b) -> p a b", a=8), eng=fw.act)
                fw.mm(psW[:, 0:512], cqT[:, 2, tc0:tc0 + 128], wukv[:, 0:512], True, True)
                fw.mm(psW[:, 512:1024], cqT[:, 2, tc0:tc0 + 128], wukv[:, 512:1024], True, True)
                yield
                kv3 = psW.re("p (h d) -> p h d", h=8)
                fw.actf(sqf.v(), psW.v(), AF.Square)
                yield
                fw.reduce(st[:, 16:24], sqf.re("p (h d) -> p h d", h=8)[:, :, 0:64])
                yield
                fw.stt(st[:, 16:24], st[:, 16:24], st[:, 7:8], st[:, 4:5].bc([128, 8]), ALU.mult, ALU.add)
                fw.ts(st[:, 16:24], st[:, 16:24], 1.0 / 96.0, ALU.mult, EPS, ALU.add)
                yield
                fw.rsqrt(st[:, 16:24], st[:, 16:24])
                yield
                fw.ts(st[:, 24:32], st[:, 16:24], st[:, 6:7], ALU.mult)
                kg3 = cbq[:, 96:192].us(1).bc([128, 8, 96])
                fw.tt(knf.v(), kv3[:, :, 0:64], st[:, 24:32].us(2).bc([128, 8, 64]), ALU.mult)
                yield
                fw.tt(kb[:, :, 0:64], knf.v(), kg3[:, :, 0:64], ALU.mult, eng=fw.pool)
                fw.tt(krn.v(), kr.v().us(1).bc([128, 8, 32]), st[:, 16:24].us(2).bc([128, 8, 32]), ALU.mult)
                yield
                fw.tt(krn.v(), krn.v(), kg3[:, :, 64:96], ALU.mult)
                rope(kb[:, :, 64:96], krn.v(), tix)
                yield
                fw.ts(VC[:, tl, :, 0:64], kv3[:, :, 64:128], st[:, 6:7], ALU.mult)
                yield
                for h in range(8):
                    fw.transpose(psT[0:96, h * 128:(h + 1) * 128], kb[:, h, :], ident_b.v(), inc=(h == 7))
                yield
                fw.copy(KT[0:96, :, tc0:tc0 + 128], psT[0:96, :].re("p (a b) -> p a b", a=8), eng=fw.act)
                yield

        def attn(b):
            zb = zb2[b % 2]
            qT = qT2[b % 2]
            nk = 4 * b + 4
            pti = 0
            for h in range(8):
                po = psO

                def qk(kt):
                    j = kt - 4 * b
                    c0 = 128 * j if j > 0 else 0
                    p = psA()
                    fw.mm(p[:, c0:512], KTb[kt // 4][0:96, h, (kt % 4) * 128:(kt % 4 + 1) * 128], qT[0:96, h, c0:512], True, True)
                    return p, c0
                nxt = qk(0)
                for kt in range(nk):
                    p, c0 = nxt
                    if kt + 1 < nk:
                        nxt = qk(kt + 1)
                    ptile = pt[pti % 3]
                    pti += 1
                    fw.actf(ptile[:, c0:512], p[:, c0:512], AF.Exp, scale=SCALE_A)
                    if kt >= 4 * b:
                        fw.memset(ptile[64:128, c0:c0 + 64], 0.0, eng=fw.pool)
                    fw.mm(po[0:65, c0:512], VCb[kt // 4][:, kt % 4, h, 0:65], ptile[:, c0:512], kt == 0, kt == nk - 1)
                    yield
                fw.recip(rden[64:65, :], po[64:65, :])
                fw.copy(ou[0:64, :], po[0:64, :])
                yield
                pb = psA()
                fw.mm(pb[0:64, :], ones_f[64:65, 0:64], rden[64:65, :], True, True)
                yield
                hp = (h % 2) * 64
                fw.tt(ytmp[hp:hp + 64, :], ou[0:64, :], pb[0:64, :], ALU.mult)
                yield
                fw.tt(ybT[hp:hp + 64, h // 2, :], ytmp[hp:hp + 64, :], zb[hp:hp + 64, h // 2, :], ALU.mult, eng=fw.pool)
                yield
            fw.dma(fw.sp, YB[s][b][s, b].re("p (a b) -> p a b", a=4), ybT.v())

        interleave([chain(0)])
        for b in range(nblk):
            gens = [attn(b)]
            if b + 1 < nblk:
                gens.append(chain(b + 1))
            interleave(gens)
        fw.barrier()

    def pass2(l, s):
        AR.reset(pass_mark)
        NSLOT = 6
        ring = [A(f"ring{i}", [128, 4096], BF16) for i in range(NSLOT)]
        hT = A("hT2", [128, 8, 512], BF16)
        ybT = A("ybT2", [128, 4, 512], BF16)
        va = A("va", [128, 4, 512], BF16)
        vc = A("vc", [128, 4, 512], BF16)
        kct = A("kct", [128, 4, 256], F32)
        gT = A("gT", [16, 512], F32)
        qk = A("qk", [128, 4, 512], F32)
        ua = A("ua", [128, 4, 512], BF16)
        za = A("za", [128, 4, 512], BF16)
        zc = A("zc", [128, 4, 512], BF16)
        uz = A("uz", [128, 4, 512], BF16)
        gt = A("gt", [128, 24, 512], BF16)
        yaT = A("yaT", [128, 4, 512], BF16)
        ycT = A("ycT", [128, 4, 512], BF16)
        mg = A("mg", [128, 8, 512], BF16)
        xres = [A(f"xres{i}", [128, 1024], F32) for i in range(4)]
        kpad = A("kpad", [128, 4, 128], BF16)
        la = A("la", [128, 256], F32)
        eb = A("eb", [128, 2, 128], F32)
        enb = A("enb", [128, 2, 128], F32)
        qt = A("qt", [128, 2, 128], BF16)
        kt_ = A("kt_", [128, 2, 128], BF16)
        eD = A("eD", [128, 256], F32)
        attm = A("attm", [128, 4, 128], BF16)
        Sf = [A(f"Sf{i}", [128, 128], F32) for i in range(2)]
        Sb = [A(f"Sb{i}", [128, 128], BF16) for i in range(2)]
        gl = A("gl", [128, 512], F32)
        xc = A("xc", [128, 512], F32)
        t1 = A("t1", [128, 512], F32)
        t2 = A("t2", [128, 512], F32)
        t3 = A("t3", [128, 512], F32)
        sqb = A("sqb", [128, 512], BF16)
        junk = A("junk2", [128, 512], BF16)

        cbc = A("cb2", [128, 1280], F32)
        sgb = A("sgb", [128, 512], F32)
        fw.dma(fw.sp, cbc.v(), CBC[l][:, 0:1280])
        fw.dma(fw.sp, sgb.v(), CBC[l][:, BC_SGB:BC_SGB + 512])
        fw.memset(kpad.v(), 0.0, eng=fw.pool)
        for i in range(2):
            fw.memset(Sf[i].v(), 0.0, eng=fw.pool)
            fw.memset(Sb[i].v(), 0.0, eng=fw.pool)

        shapes = [(a_, c_) for (_, a_, c_) in slab_srcs(l)]
        sched = [(j, shapes[j][0], shapes[j][1]) for _ in range(nblk) for j in range(NSL)]
        state = {"issued": 0, "next": 0, "done": 0}

        def rel(n=1):
            state["done"] += n
            issue_until(state["done"] + NSLOT)

        def issue_until(n):
            while state["issued"] < min(n, len(sched)):
                i = state["issued"]
                j, a, c = sched[i]
                slot = ring[i % NSLOT]
                fw.dma(fw.sp, slot[:, 0:a * c], WS[j][j, :, 0:a * c])
                state["issued"] += 1

        def slab():
            i = state["next"]
            assert i < state["done"] + NSLOT
            issue_until(i + 1)
            state["next"] += 1
            _, a, c = sched[i]
            return ring[i % NSLOT][:, 0:a * c].re("p (a c) -> p a c", a=a)

        issue_until(NSLOT)

        psG = ring_alloc([psWa, psWb])

        def fm_group(w, c0, m=128, alloc=None):
            p = (alloc or ps)()
            for kc in range(8):
                fw.mm(p[0:m, :], w[:, kc, c0:c0 + m], hT[:, kc, :], kc == 0, kc == 7)
            return p

        for b in range(nblk):
            if b == 0:
                fw.dma(fw.sp, hT.v(), HT[s][b][s, b].re("p (a b) -> p a b", a=8))
            fw.dma(fw.sp, ybT.v(), YB[s][b][s, b].re("p (a b) -> p a b", a=4))
            for tl in range(4):
                xin = X if l == 0 else Y[s][b * 4 + tl]
                fw.dma(fw.sp, xres[tl].v(), xin[s, b * 512 + tl * 128:b * 512 + (tl + 1) * 128, :])
            w = slab()
            for tl in range(4):
                p = ps()
                for kc in range(8):
                    fw.mm(p.v(), hT[:, kc, tl * 128:(tl + 1) * 128], w[:, kc, :], kc == 0, kc == 7)
                fw.actf(gl.v(), p.v(), AF.Gelu, accum_out=st[:, 0:1])
                fw.ts(st[:, 1:2], st[:, 0:1], 1.0 / 512.0, ALU.mult)
                fw.ts(xc.v(), gl.v(), st[:, 1:2], ALU.subtract)
                fw.actf(junk.v(), xc.v(), AF.Square, accum_out=st[:, 2:3])
                fw.ts(st[:, 3:4], st[:, 2:3], 1.0 / 512.0, ALU.mult, EPS, ALU.add)
                fw.rsqrt(st[:, 3:4], st[:, 3:4])
                fw.stt(t1.v(), xc.v(), st[:, 3:4], cbc[:, BC_LNG:BC_LNG + 512], ALU.mult, ALU.mult)
                fw.tt(va[:, tl, :], t1.v(), cbc[:, BC_LNB:BC_LNB + 512], ALU.add, eng=fw.pool)
            rel()
            w = slab()
            for tl in range(4):
                p = ps()
                for kc in range(8):
                    fw.mm(p.v(), hT[:, kc, tl * 128:(tl + 1) * 128], w[:, kc, :], kc == 0, kc == 7)
                fw.copy(kct[:, tl, :], p[:, 0:256], eng=fw.act)
                fw.copy(vc[:, tl, 0:256], p[:, 256:512])
            rel()
            w = slab()
            for tl in range(4):
                p = ps()
                for kc in range(8):
                    fw.mm(p[:, 0:256], hT[:, kc, tl * 128:(tl + 1) * 128], w[:, kc, :], kc == 0, kc == 7)
                fw.copy(vc[:, tl, 256:512], p[:, 0:256], eng=fw.act)
            chk("p2.1")
            rel()
            w = slab()
            p = fm_group(w, 0, 16)
            fw.copy(gT.v(), p[0:16, :])
            rel()
            w = slab()
            for g in range(4):
                p = fm_group(w, g * 128)
                fw.copy(qk[:, g, :], p.v(), eng=fw.act)
            rel()
            w = slab()
            for g in range(4):
                p = fm_group(w, g * 128)
                fw.actf(zc[:, g, :], p.v(), AF.Silu)
            rel()
            w = slab()
            for g in range(4):
                p = fm_group(w, g * 128)
                fw.actf(ua[:, g, :], p.v(), AF.Gelu)
            rel()
            w = slab()
            for g in range(4):
                p = fm_group(w, g * 128)
                fw.actf(za[:, g, :], p.v(), AF.Silu)
            fw.tt(uz.v(), ua.v(), za.v(), ALU.mult, eng=fw.pool)
            def gates_gen():
                for i6 in range(6):
                    rel()
                    w = slab()
                    for g in range(4):
                        gi = i6 * 4 + g
                        p = fm_group(w, g * 128, alloc=psG)
                        yield
                        fw.actf(gt[:, gi, :], p.v(), AF.Sigmoid, bias=cpp[:, PP_BGATE + gi:PP_BGATE + gi + 1])
                        yield
            def gla_gen():
                for tl in range(4):
                    tc0 = tl * 128
                    p = ps()
                    for g in range(4):
                        fw.mm(p[:, g * 128:(g + 1) * 128], va[:, tl, g * 128:(g + 1) * 128], sgw[:, g, :], True, True, inc=(g == 3))
                    fw.tt(t1.v(), p.v(), sgb.v(), ALU.add)
                    yield
                    fw.tt(yaT[:, :, tc0:tc0 + 128], t1.re("p (g i) -> p g i", g=4), uz[:, :, tc0:tc0 + 128], ALU.mult, eng=fw.pool)
                    yield
                for tl in range(4):
                    tc0 = tl * 128
                    p = ps()
                    fw.mm(p[:, 0:256], gT[0:16, tc0:tc0 + 128], wgu[0:16, :], True, False, inc=False)
                    fw.mm(p[:, 0:256], ones_f[0:1, 0:128], bgu[0:1, :], False, True)
                    fw.actf(la.v(), p[:, 0:256], AF.Exp, scale=-1.0)
                    yield
                    fw.actf(la.v(), la.v(), AF.Ln, bias=1.0)
                    yield
                    fw.ts(la.v(), la.v(), -1.0 / 16.0, ALU.mult)
                    yield
                    pbT = ps()
                    for fc in range(2):
                        fw.mm(pbT[:, fc * 128:(fc + 1) * 128], la[:, fc * 128:(fc + 1) * 128], Tm_f, True, True, inc=(fc == 1))
                    pD = ps()
                    fw.mm(pD[:, 0:256], Tu_f, la.v(), True, True)
                    fw.actf(eb.re("p a b -> p (a b)"), pbT[:, 0:256], AF.Exp)
                    yield
                    fw.actf(enb.re("p a b -> p (a b)"), pbT[:, 0:256], AF.Exp, scale=-1.0)
                    yield
                    fw.actf(eD.v(), pD[:, 0:256], AF.Exp)
                    yield
                    fw.stt(qt.v(), qk[:, 0:2, tc0:tc0 + 128], 0.125, eb.v(), ALU.mult, ALU.mult)
                    yield
                    fw.tt(kt_.v(), qk[:, 2:4, tc0:tc0 + 128], enb.v(), ALU.mult, eng=fw.pool)
                    yield
                    for h in range(4):
                        o = (h % 2) * 64
                        fw.tt(kpad[:, h, o:o + 64], kct[:, tl, h * 64:(h + 1) * 64], eD[:, h * 64:(h + 1) * 64], ALU.mult,
                              eng=(fw.pool if h % 2 else fw.dve))
                    po = psO
                    for h in range(4):
                        fc = h // 2
                        o = (h % 2) * 64
                        pa = ps()
                        fw.mm(pa[:, 0:128], kt_[o:o + 64, fc, :], qt[o:o + 64, fc, :], True, True)
                        fw.tt(attm[:, h, :], pa[:, 0:128], Tm_b.v(), ALU.mult)
                        yield
                    for c in range(2):
                        r0 = c * 64
                        for h in range(4):
                            fc = h // 2
                            o = (h % 2) * 64
                            dst = po[:, h * 128 + r0:h * 128 + r0 + 64]
                            fw.mm(dst, vc[:, tl, h * 128:(h + 1) * 128], attm[:, h, r0:r0 + 64], True, False, inc=False)
                            fw.mm(dst, Sb[fc][o:o + 64, :], qt[o:o + 64, fc, r0:r0 + 64], False, True, inc=True)
                        for fc in range(2):
                            pk = ps()
                            for hh in range(2):
                                h = fc * 2 + hh
                                fw.mm(pk[:, 0:128], kpad[r0:r0 + 64, h, :], vc[r0:r0 + 64, tl, h * 128:(h + 1) * 128], hh == 0, hh == 1)
                            dec = eb[:, fc, r0 + 63:r0 + 64]
                            fw.stt(Sf[fc].v(), Sf[fc].v(), dec, pk[:, 0:128], ALU.mult, ALU.add)
                            yield
                            fw.copy(Sb[fc].v(), Sf[fc].v(), eng=fw.act)
                            yield
                    fw.actf(sqb.v(), po.v(), AF.Square)
                    yield
                    pss = ps()
                    fw.mm(pss.v(), ones_b.v(), sqb.v(), True, True)
                    fw.ts(t2.v(), pss.v(), 1.0 / 128.0, ALU.mult, EPS, ALU.add)
                    yield
                    fw.rsqrt(t2.v(), t2.v())
                    yield
                    fw.tt(t3.v(), po.v(), t2.v(), ALU.mult)
                    yield
                    fw.stt(ycT[:, :, tc0:tc0 + 128], t3.re("p (h i) -> p h i", h=4), cpp[:, PP_OG:PP_OG + 1],
                           zc[:, :, tc0:tc0 + 128], ALU.mult, ALU.mult)
            interleave([gates_gen(), gla_gen()])
            if b + 1 < nblk:
                fw.dma(fw.sp, hT.v(), HT[s][b + 1][s, b + 1].re("p (a b) -> p a b", a=8))
            chk("gla")
            rel()
            wb = [slab() for _ in range(3)]
            ys = [yaT, ybT, ycT]
            for og in range(8):
                pp = []
                for i in range(3):
                    p = ps()
                    for kc in range(4):
                        fw.mm(p.v(), wb[i][:, kc, og * 128:(og + 1) * 128], ys[i][:, kc, :], kc == 0, kc == 3)
                    pp.append(p)
                fw.tt(t1.v(), pp[0].v(), gt[:, og, :], ALU.mult)
                fw.tt(t2.v(), pp[1].v(), gt[:, 8 + og, :], ALU.mult)
                fw.tt(t3.v(), pp[2].v(), gt[:, 16 + og, :], ALU.mult)
                fw.tt(t1.v(), t1.v(), t2.v(), ALU.add, eng=fw.pool)
                fw.tt(mg[:, og, :], t1.v(), t3.v(), ALU.add, eng=fw.pool)
            chk("p2.4")
            rel(3)
            wo = [slab() for _ in range(2)]
            for tl in range(4):
                r0 = b * 512 + tl * 128
                xt = xres[tl]
                for hf in range(2):
                    p = ps()
                    for kc in range(8):
                        fw.mm(p.v(), mg[:, kc, tl * 128:(tl + 1) * 128], wo[hf][:, kc, :], kc == 0, kc == 7)
                    fw.tt(xt[:, hf * 512:(hf + 1) * 512], xt[:, hf * 512:(hf + 1) * 512], p.v(), ALU.add)
                fw.dma(fw.sp, Y[s][b * 4 + tl][s, r0:r0 + 128, :], xt.v())
            rel(2)
        fw.barrier()

    try:
        chk("setup")
        for l in range(depth):
            convert_layer(l)
            chk("consts")
            for s in range(nseq):
                pass1(l, s)
                chk("pass1")
                pass2(l, s)
    except _Stop:
        pass
    fw.barrier()
    return nc, fw


def _consts():
    j = np.arange(128)[:, None]
    i = np.arange(128)[None, :]
    same = (j // 64) == (i // 64)
    Tm = (same & (j <= i)).astype(np.float32)
    Tu = (same & (j > i)).astype(np.float32)
    half = 16
    inv = (np.float32(1.0) / np.power(np.float32(10000.0),
                                      np.arange(half, dtype=np.float32) * np.float32(2.0) / np.float32(32))).astype(np.float32)
    cst = np.concatenate([np.eye(128, dtype=np.float32), Tm, Tu, np.broadcast_to(inv[None, :], (128, 16))], axis=1)
    return np.ascontiguousarray(cst)


def _pack(inputs, depth):
    f = lambda k: np.asarray(inputs[k], dtype=np.float32)
    cpp = np.zeros((depth, 128, NPP), np.float32)
    cbc = np.zeros((depth, 128, NBC), np.float32)
    for l in range(depth):
        cpp[l, :, PP_NORM:PP_NORM + 8] = f("norm_g")[l].reshape(8, 128).T
        cpp[l, :, PP_CQG:PP_CQG + 2] = f("mla_cq_g")[l].reshape(2, 128).T
        cpp[l, :, PP_CKVG] = f("mla_ckv_g")[l]
        cpp[l, :, PP_BGATE:PP_BGATE + 24] = f("b_gate")[l].reshape(24, 128).T
        cpp[l, :, PP_OG] = f("gla_o_g")[l]
        cbc[l, :, BC_LNG:BC_LNG + 512] = f("sg_ln_g")[l][None, :]
        cbc[l, :, BC_LNB:BC_LNB + 512] = f("sg_ln_b")[l][None, :]
        cbc[l, :, BC_BGU:BC_BGU + 256] = f("gla_b_gate")[l][None, :]
        cbc[l, :, BC_QG:BC_QG + 96] = f("mla_q_g")[l][None, :]
        cbc[l, :, BC_KG:BC_KG + 96] = f("mla_k_g")[l][None, :]
        cbc[l, :, BC_SGB:BC_SGB + 512] = f("sg_b")[l].reshape(512)[None, :]
    return cpp, cbc


_CACHE = {}


def run(inputs, n_cores, nseq, nblk, depth):
    key = (nseq, nblk, depth)
    if key not in _CACHE:
        _CACHE[key] = build(nseq, nblk, depth)
    nc, fw = _CACHE[key]
    S = nblk * 512
    x = np.asarray(inputs["x"], np.float32)
    pos = np.asarray(inputs["positions"], np.int32)
    cpp, cbc = _pack(inputs, depth)
    cst = _consts()
    f = lambda k: np.ascontiguousarray(np.asarray(inputs[k], np.float32)[:depth])
    shared = {
        "w_in": f("w_in"), "w_uq": f("mla_w_uq"), "w_ukv": f("mla_w_ukv"), "w_gu": f("gla_w_gate"),
        "b_gu": f("gla_b_gate").reshape(depth, 1, 256),
        "sg_wT": np.ascontiguousarray(f("sg_w").transpose(0, 1, 3, 2)),
        "w_branch": f("w_branch"), "w_out": f("w_out"), "cpp": cpp, "cbc": cbc, "cst": cst,
    }
    in_maps = []
    for c in range(n_cores):
        xs = np.ascontiguousarray(x[c * nseq:(c + 1) * nseq, :S])
        ps_ = pos[c * nseq:(c + 1) * nseq, :S]
        posT = np.ascontiguousarray(ps_.reshape(nseq * nblk * 4, 128).T)
        m = dict(shared)
        m["x"] = xs
        m["posT"] = posT
        in_maps.append(m)
    res = run_bass_kernel_spmd(nc, in_maps, core_ids=list(range(n_cores)))
    return np.concatenate([r["y"] for r in res.results], axis=0)


def kernel(**inputs):
    return run(inputs, N_CORES, 2, 8, 4)
```
